# Optimizing a Trainium2 kernel written in Bass

```python
import jax
import jax.numpy as jnp
from jax import lax
import numpy as np

D_MODEL = 2048
BATCH = 16
SEQ = 2048
DEPTH = 1

GRID_W = 64
CTX_LEN = 256
EPS = 1e-6
N_MOD = 6

GLA_HEADS = 4
GLA_DK = D_MODEL // 2
GLA_DV = D_MODEL
GLA_HK = GLA_DK // GLA_HEADS
GLA_HV = GLA_DV // GLA_HEADS
GLA_RANK = 16
GLA_NORMALIZER = 16.0
GLA_CHUNK = 64

SSD_DI = 2 * D_MODEL
SSD_HEADDIM = 64
SSD_HEADS = SSD_DI // SSD_HEADDIM
SSD_GROUPS = 8
SSD_HPG = SSD_HEADS // SSD_GROUPS
SSD_STATE = 128
SSD_CONV = 3
SSD_CHUNK = 128
SSD_CONV_CH = SSD_DI + 2 * SSD_GROUPS * SSD_STATE

PEER_HEADS = 8
PEER_NKEYS = 128
PEER_EXPERTS = PEER_NKEYS * PEER_NKEYS
PEER_DKEY = 256
PEER_TOPK = 16
PEER_BLOCK = 128

IN_SIZES = (GLA_DK, GLA_DK, GLA_DV, GLA_DV, GLA_RANK, GLA_RANK,
            SSD_DI, SSD_CONV_CH, SSD_HEADS, SSD_HEADS, 2 * D_MODEL)
IN_COLS = sum(IN_SIZES)
IN_SPLITS = [int(s) for s in np.cumsum(IN_SIZES)[:-1]]

kernel_name = 'hybrid_gla_ssd_peer_prefix_block'


def rmsnorm(x, w):
    xf = x.astype(jnp.float32)
    y = xf * lax.rsqrt(jnp.mean(xf * xf, axis=-1, keepdims=True) + EPS)
    return (y * w.astype(jnp.float32)).astype(x.dtype)


def modulate(h, shift, scale):
    return h * (1.0 + scale) + shift


def _chunks(t, size):
    b, l = t.shape[:2]
    return jnp.moveaxis(t.reshape(b, l // size, size, *t.shape[2:]), 1, 0)


def _unchunk(t):
    t = jnp.moveaxis(t, 0, 1)
    return t.reshape(t.shape[0], t.shape[1] * t.shape[2], *t.shape[3:])


def _flip(t):
    return jnp.flip(t, axis=1)


def gla_scan(q, k, v, g, s0):
    mask = jnp.tril(jnp.ones((GLA_CHUNK, GLA_CHUNK), dtype=bool))

    def step(s, inp):
        qc, kc, vc, gc = inp
        b = jnp.cumsum(gc, axis=1)
        b_last = b[:, -1]
        qd = qc * jnp.exp(b)
        kd = kc * jnp.exp(-b)
        a = jnp.where(mask, jnp.einsum('bihk,bjhk->bhij', qd, kd), 0.0)
        o = jnp.einsum('bhij,bjhv->bihv', a, vc) + jnp.einsum('bihk,bhkv->bihv', qd, s)
        kt = kc * jnp.exp(b_last[:, None] - b)
        s = s * jnp.exp(b_last)[..., None] + jnp.einsum('bjhk,bjhv->bhkv', kt, vc)
        return s, o

    s, o = lax.scan(step, s0, (_chunks(q, GLA_CHUNK), _chunks(k, GLA_CHUNK),
                               _chunks(v, GLA_CHUNK), _chunks(g, GLA_CHUNK)))
    return _unchunk(o), s


def ssd_scan(xs, dt, a, bm, cm, s0):
    mask = jnp.tril(jnp.ones((SSD_CHUNK, SSD_CHUNK), dtype=bool))

    def step(s, inp):
        xc, dtc, bc, cc = inp
        cum = jnp.cumsum(dtc * a, axis=1)
        cum_t = jnp.moveaxis(cum, 1, -1)
        seg = cum_t[..., :, None] - cum_t[..., None, :]
        decay = jnp.exp(jnp.where(mask, seg, -jnp.inf))
        cb = jnp.einsum('bign,bjgn->bgij', cc, bc)
        xdt = xc * dtc[..., None]
        y = jnp.einsum('bghij,bjghp->bighp', cb[:, :, None] * decay, xdt)
        y = y + jnp.einsum('bign,bghnp->bighp', cc, s) * jnp.exp(cum)[..., None]
        c_last = cum[:, -1]
        w = jnp.exp(c_last[:, None] - cum)[..., None] * xdt
        s = s * jnp.exp(c_last)[..., None, None] + jnp.einsum('bjgn,bjghp->bghnp', bc, w)
        return s, y

    s, y = lax.scan(step, s0, (_chunks(xs, SSD_CHUNK), _chunks(dt, SSD_CHUNK),
                               _chunks(bm, SSD_CHUNK), _chunks(cm, SSD_CHUNK)))
    return _unchunk(y), s


def dwconv2d(t, rows, w, b):
    bn, l, ch = t.shape
    img = t.reshape(bn, rows, l // rows, ch)
    y = lax.conv_general_dilated(
        img, w.reshape(SSD_CONV, SSD_CONV, 1, ch).astype(t.dtype), (1, 1), 'SAME',
        dimension_numbers=('NHWC', 'HWIO', 'NHWC'), feature_group_count=ch)
    return y.reshape(bn, l, ch) + b


def branch_core(h, rows, p, init):
    bn, l, _ = h.shape
    f32 = jnp.float32
    (q, k, v, r, lr_f, lr_b, z, xbc, dt_f, dt_b, gl) = jnp.split(h @ p['w_in'], IN_SPLITS, axis=-1)

    qh = q.reshape(bn, l, GLA_HEADS, GLA_HK).astype(f32) * GLA_HK ** -0.5
    kh = k.reshape(bn, l, GLA_HEADS, GLA_HK).astype(f32)
    vh = v.reshape(bn, l, GLA_HEADS, GLA_HV).astype(f32)

    def log_decay(lr, w2, b2):
        logit = (lr @ w2 + b2).astype(f32)
        return (jax.nn.log_sigmoid(logit) / GLA_NORMALIZER).reshape(bn, l, GLA_HEADS, GLA_HK)

    g_f = log_decay(lr_f, p['w_lr2_f'], p['b_lr_f'])
    g_b = log_decay(lr_b, p['w_lr2_b'], p['b_lr_b'])

    xbc = jax.nn.silu(dwconv2d(xbc, rows, p['conv_w'], p['conv_b']))
    xs, bm, cm = jnp.split(xbc, [SSD_DI, SSD_DI + SSD_GROUPS * SSD_STATE], axis=-1)
    xs = xs.reshape(bn, l, SSD_GROUPS, SSD_HPG, SSD_HEADDIM).astype(f32)
    bm = bm.reshape(bn, l, SSD_GROUPS, SSD_STATE).astype(f32)
    cm = cm.reshape(bn, l, SSD_GROUPS, SSD_STATE).astype(f32)

    def dt_act(dt, bias):
        return jax.nn.softplus((dt + bias).astype(f32)).reshape(bn, l, SSD_GROUPS, SSD_HPG)

    dtf = dt_act(dt_f, p['dt_bias_f'])
    dtb = dt_act(dt_b, p['dt_bias_b'])
    a_f = -jnp.exp(p['a_log_f'].astype(f32)).reshape(SSD_GROUPS, SSD_HPG)
    a_b = -jnp.exp(p['a_log_b'].astype(f32)).reshape(SSD_GROUPS, SSD_HPG)

    if init is None:
        zg = jnp.zeros((bn, GLA_HEADS, GLA_HK, GLA_HV), f32)
        zs = jnp.zeros((bn, SSD_GROUPS, SSD_HPG, SSD_STATE, SSD_HEADDIM), f32)
        init = (zg, zg, zs, zs)
    s_gf, s_gb, s_sf, s_sb = init

    o_f, s_gf = gla_scan(qh, kh, vh, g_f, s_gf)
    o_b, s_gb = gla_scan(_flip(qh), _flip(kh), _flip(vh), _flip(g_b), s_gb)
    o_gla = o_f + _flip(o_b)

    y_f, s_sf = ssd_scan(xs, dtf, a_f, bm, cm, s_sf)
    y_b, s_sb = ssd_scan(_flip(xs), _flip(dtb), a_b, _flip(bm), _flip(cm), s_sb)
    d = p['d_skip'].astype(f32).reshape(SSD_GROUPS, SSD_HPG)[..., None]
    y_ssd = y_f + _flip(y_b) + d * xs
    return (o_gla, r, y_ssd, z, gl), (s_gf, s_gb, s_sf, s_sb)


def branch_out(pieces, p):
    o_gla, r, y_ssd, z, gl = pieces
    bn, l = r.shape[:2]
    dtype = r.dtype
    f32 = jnp.float32
    o = o_gla * lax.rsqrt(jnp.mean(o_gla * o_gla, axis=-1, keepdims=True) + EPS)
    o = (o * p['gla_norm_w'].astype(f32)).reshape(bn, l, GLA_DV) * jax.nn.silu(r.astype(f32))
    y_a = o.astype(dtype) @ p['w_gla_out']
    y = y_ssd.reshape(bn, l, SSD_DI) * jax.nn.silu(z.astype(f32))
    y = y.reshape(bn, l, SSD_GROUPS, SSD_DI // SSD_GROUPS)
    y = y * lax.rsqrt(jnp.mean(y * y, axis=-1, keepdims=True) + EPS)
    y = y.reshape(bn, l, SSD_DI) * p['ssd_norm_w'].astype(f32)
    y_b = y.astype(dtype) @ p['w_ssd_out']
    ga, gb = jnp.split(jax.nn.sigmoid(gl + p['b_gate']), 2, axis=-1)
    return (ga * y_a + gb * y_b) @ p['w_o']


def peer(h, w_q, sub_keys, u, v):
    bn, l, d = h.shape
    t = h.reshape(-1, d)
    n_tok = t.shape[0]
    q = (t @ w_q).astype(jnp.float32).reshape(n_tok, PEER_HEADS, 2, PEER_DKEY // 2)
    s = jnp.einsum('thpd,hpkd->thpk', q, sub_keys.astype(jnp.float32))
    sv, si = lax.top_k(s, PEER_TOPK)
    cand = sv[:, :, 0, :, None] + sv[:, :, 1, None, :]
    cv, ci = lax.top_k(cand.reshape(n_tok, PEER_HEADS, PEER_TOPK * PEER_TOPK), PEER_TOPK)
    i1 = jnp.take_along_axis(si[:, :, 0], ci // PEER_TOPK, axis=-1)
    i2 = jnp.take_along_axis(si[:, :, 1], ci % PEER_TOPK, axis=-1)
    idx = i1 * PEER_NKEYS + i2
    gate = jax.nn.softmax(cv, axis=-1)
    nb = n_tok // PEER_BLOCK

    def block(args):
        tb, ib, gb = args
        ue = jnp.take(u, ib, axis=0)
        act = jax.nn.gelu(jnp.einsum('td,thkd->thk', tb, ue).astype(jnp.float32), approximate=False)
        ve = jnp.take(v, ib, axis=0)
        return jnp.einsum('thk,thkd->td', (act * gb).astype(tb.dtype), ve)

    y = lax.map(block, (t.reshape(nb, PEER_BLOCK, d),
                        idx.reshape(nb, PEER_BLOCK, PEER_HEADS, PEER_TOPK),
                        gate.reshape(nb, PEER_BLOCK, PEER_HEADS, PEER_TOPK)))
    return y.reshape(bn, l, d)


def setup_inputs(seed: int = 0) -> dict:
    key = jax.random.key(seed)
    ks = iter(jax.random.split(key, 40))
    f32 = jnp.float32

    def nrm(shape, scale):
        return jax.random.normal(next(ks), shape, f32) * scale

    def dt_bias(shape):
        dt = jnp.exp(jax.random.uniform(next(ks), shape, f32, float(np.log(1e-3)), float(np.log(1e-1))))
        return dt + jnp.log(-jnp.expm1(-dt))

    def a_log(shape):
        return jnp.log(jax.random.uniform(next(ks), shape, f32, 1.0, 16.0))

    D = D_MODEL
    return {
        'x': nrm((BATCH, SEQ, D), 1.0),
        'c': nrm((BATCH, D), 1.0),
        'ctx': nrm((BATCH, CTX_LEN, D), 1.0),
        'c_ctx': nrm((D,), 1.0),
        'w_ada': nrm((DEPTH, D, N_MOD * D), 0.5 * D ** -0.5),
        'b_ada': nrm((DEPTH, N_MOD * D), 0.01),
        'norm1_w': 1.0 + nrm((DEPTH, D), 0.01),
        'w_in': nrm((DEPTH, D, IN_COLS), D ** -0.5),
        'b_gate': nrm((DEPTH, 2 * D), 0.01),
        'w_lr2_f': nrm((DEPTH, GLA_RANK, GLA_DK), GLA_RANK ** -0.5),
        'b_lr_f': nrm((DEPTH, GLA_DK), 0.01),
        'w_lr2_b': nrm((DEPTH, GLA_RANK, GLA_DK), GLA_RANK ** -0.5),
        'b_lr_b': nrm((DEPTH, GLA_DK), 0.01),
        'gla_norm_w': 1.0 + nrm((DEPTH, GLA_HV), 0.01),
        'w_gla_out': nrm((DEPTH, GLA_DV, D), GLA_DV ** -0.5),
        'conv_w': nrm((DEPTH, SSD_CONV, SSD_CONV, SSD_CONV_CH), 1.0 / SSD_CONV),
        'conv_b': nrm((DEPTH, SSD_CONV_CH), 0.01),
        'a_log_f': a_log((DEPTH, SSD_HEADS)),
        'a_log_b': a_log((DEPTH, SSD_HEADS)),
        'dt_bias_f': dt_bias((DEPTH, SSD_HEADS)),
        'dt_bias_b': dt_bias((DEPTH, SSD_HEADS)),
        'd_skip': 1.0 + nrm((DEPTH, SSD_HEADS), 0.01),
        'ssd_norm_w': 1.0 + nrm((DEPTH, SSD_DI), 0.01),
        'w_ssd_out': nrm((DEPTH, SSD_DI, D), SSD_DI ** -0.5),
        'w_o': nrm((DEPTH, D, D), D ** -0.5),
        'norm2_w': 1.0 + nrm((DEPTH, D), 0.01),
        'peer_wq': nrm((DEPTH, D, PEER_HEADS * PEER_DKEY), D ** -0.5),
        'peer_keys': nrm((DEPTH, PEER_HEADS, 2, PEER_NKEYS, PEER_DKEY // 2), (PEER_DKEY // 2) ** -0.5),
        'peer_u': nrm((DEPTH, PEER_EXPERTS, D), D ** -0.5),
        'peer_v': nrm((DEPTH, PEER_EXPERTS, D), PEER_HEADS ** -0.5),
        'final_norm_w': 1.0 + nrm((D,), 0.01),
    }


def reference(x, c, ctx, c_ctx, w_ada, b_ada, norm1_w, w_in, b_gate, w_lr2_f, b_lr_f,
              w_lr2_b, b_lr_b, gla_norm_w, w_gla_out, conv_w, conv_b, a_log_f, a_log_b,
              dt_bias_f, dt_bias_b, d_skip, ssd_norm_w, w_ssd_out, w_o, norm2_w,
              peer_wq, peer_keys, peer_u, peer_v, final_norm_w):
    rows = x.shape[1] // GRID_W
    for layer in range(DEPTH):
        p = {
            'w_in': w_in[layer], 'b_gate': b_gate[layer],
            'w_lr2_f': w_lr2_f[layer], 'b_lr_f': b_lr_f[layer],
            'w_lr2_b': w_lr2_b[layer], 'b_lr_b': b_lr_b[layer],
            'gla_norm_w': gla_norm_w[layer], 'w_gla_out': w_gla_out[layer],
            'conv_w': conv_w[layer], 'conv_b': conv_b[layer],
            'a_log_f': a_log_f[layer], 'a_log_b': a_log_b[layer],
            'dt_bias_f': dt_bias_f[layer], 'dt_bias_b': dt_bias_b[layer],
            'd_skip': d_skip[layer], 'ssd_norm_w': ssd_norm_w[layer],
            'w_ssd_out': w_ssd_out[layer], 'w_o': w_o[layer],
        }
        mod_x = jnp.split((jax.nn.silu(c) @ w_ada[layer] + b_ada[layer])[:, None, :], N_MOD, axis=-1)
        mod_c = jnp.split((jax.nn.silu(c_ctx) @ w_ada[layer] + b_ada[layer])[None, None, :], N_MOD, axis=-1)

        hc = modulate(rmsnorm(ctx, norm1_w[layer]), mod_c[0], mod_c[1])
        hx = modulate(rmsnorm(x, norm1_w[layer]), mod_x[0], mod_x[1])
        core_c, ctx_states = branch_core(hc, 1, p, None)
        core_x, _ = branch_core(hx, rows, p, ctx_states)
        x = x + mod_x[2] * branch_out(core_x, p)

        hx2 = modulate(rmsnorm(x, norm2_w[layer]), mod_x[3], mod_x[4])
        x = x + mod_x[5] * peer(hx2, peer_wq[layer], peer_keys[layer], peer_u[layer], peer_v[layer])

        if layer < DEPTH - 1:
            ctx = ctx + mod_c[2] * branch_out(core_c, p)
            hc2 = modulate(rmsnorm(ctx, norm2_w[layer]), mod_c[3], mod_c[4])
            ctx = ctx + mod_c[5] * peer(hc2, peer_wq[layer], peer_keys[layer], peer_u[layer], peer_v[layer])
    return rmsnorm(x, final_norm_w)
```

```python
from contextlib import ExitStack
import numpy as np
import concourse.bass as bass
import concourse.mybir as mybir
from concourse.bass_utils import run_bass_kernel_spmd

F32 = mybir.dt.float32
BF16 = mybir.dt.bfloat16
AF = mybir.ActivationFunctionType
ALU = mybir.AluOpType

N_DMA_SEMS = 24
D = 2048
KC = 16
SEQ = 2048
CTXL = 256
LTOT = SEQ + CTXL
NT = LTOT // 128
NTC = CTXL // 128
EPS = 1e-6
C_Q, C_K, C_V, C_R, C_LRF, C_LRB, C_Z = 0, 1024, 2048, 4096, 6144, 6160, 6176
C_XS, C_B, C_C, C_DT, C_GL = 10272, 14368, 15392, 16416, 16544
IN_COLS = 20640


class Sched:
    ENGS = ("pe", "act", "dve", "pool", "sp")

    def __init__(self, nc, es):
        self.nc = nc
        self.q = {e: [] for e in self.ENGS}
        self.sem = {e: es.enter_context(nc.semaphore("s_" + e)) for e in self.ENGS}
        self.cnt = {e: 0 for e in self.ENGS}
        self.seen = {e: {} for e in self.ENGS}
        self.dsem = [es.enter_context(nc.semaphore("d_%d" % i)) for i in range(N_DMA_SEMS)]
        self.dcnt = [0] * N_DMA_SEMS
        self.dnext = 0
        self.last_w = {}
        self.readers = {}
        self.n_inst = 0

    def _deps(self, reads, writes):
        deps = set()
        for b in reads:
            t = self.last_w.get(b)
            if t is not None:
                deps.add(t)
        for b in writes:
            t = self.last_w.get(b)
            if t is not None:
                deps.add(t)
            for r in self.readers.get(b, ()):
                deps.add(r)
        return deps

    def _emit_waits(self, e, deps, skip_self_pe=True):
        seen = self.seen[e]
        best = {}
        for (k, v) in deps:
            if e == "pe" and k == "pe" and skip_self_pe:
                continue
            if seen.get(k, 0) >= v:
                continue
            if best.get(k, 0) < v:
                best[k] = v
        for k, v in best.items():
            seen[k] = v
            sem = self.sem[k] if isinstance(k, str) else self.dsem[k]
            self.q[e].append(("wait", sem, v))

    def _record(self, tok, reads, writes):
        for b in writes:
            self.last_w[b] = tok
            self.readers[b] = []
        for b in reads:
            if b in writes:
                continue
            self.readers.setdefault(b, []).append(tok)

    def op(self, e, fn, reads=(), writes=()):
        deps = self._deps(reads, writes)
        self._emit_waits(e, deps)
        self.cnt[e] += 1
        tok = (e, self.cnt[e])
        self.q[e].append(("op", fn, self.sem[e]))
        self._record(tok, reads, writes)
        self.n_inst += 1
        return tok

    def dma(self, fn, reads=(), writes=(), q="sp"):
        deps = self._deps(reads, writes)
        i = self.dnext
        self.dnext = (self.dnext + 1) % N_DMA_SEMS
        if self.dcnt[i] > 0:
            deps.add((i, self.dcnt[i]))
        self._emit_waits(q, deps, skip_self_pe=False)
        self.dcnt[i] += 16
        tok = (i, self.dcnt[i])
        self.q[q].append(("dma", fn, self.dsem[i]))
        self._record(tok, reads, writes)
        self.n_inst += 1
        return tok

    def wait_all(self, e):
        deps = set()
        for k in self.ENGS:
            if self.cnt[k] > 0:
                deps.add((k, self.cnt[k]))
        for i in range(N_DMA_SEMS):
            if self.dcnt[i] > 0:
                deps.add((i, self.dcnt[i]))
        self._emit_waits(e, deps, skip_self_pe=False)

    def barrier(self):
        for e in self.ENGS:
            self.wait_all(e)
        self.last_w = {}
        self.readers = {}

    def emit(self):
        nc = self.nc
        with nc.Block() as block:
            def run(e):
                def body(eng):
                    for it in self.q[e]:
                        if it[0] == "wait":
                            eng.wait_ge(it[1], it[2])
                        elif it[0] == "op":
                            it[1](eng).then_inc(it[2], 1)
                        else:
                            it[1](eng).then_inc(it[2], 16)
                return body
            block.tensor(run("pe"))
            block.scalar(run("act"))
            block.vector(run("dve"))
            block.gpsimd(run("pool"))
            block.sync(run("sp"))


class K:
    def __init__(self, nb=2, stages="0ABCDE", dbg=None):
        self.nb = nb
        self.stages = stages
        self.dbg = dbg or {}
        self.uid = 0

    def sb(self, es, name, shape, dt=F32):
        self.uid += 1
        return es.enter_context(self.nc.sbuf_tensor("%s_%d" % (name, self.uid), list(shape), dt))

    def ps_next(self):
        i = self.ps_rot[self.ps_i % len(self.ps_rot)]
        self.ps_i += 1
        return i

    def build(self):
        nc = bass.Bass("TRN2", target_bir_lowering=False)
        self.nc = nc
        nb = self.nb

        def din(name, shape, dt=F32):
            return nc.dram_tensor(name, list(shape), dt, kind="ExternalInput").ap()

        io = {}
        io["x"] = din("x", [nb, SEQ, D])
        io["ctx"] = din("ctx", [nb, CTXL, D])
        io["cT"] = din("cT", [128, KC, 3])
        io["w_ada"] = din("w_ada", [D, 6 * D])
        io["b_adaT"] = din("b_adaT", [128, 96])
        io["n1T"] = din("n1T", [128, KC])
        io["n2T"] = din("n2T", [128, KC])
        io["w_in"] = din("w_in", [D, IN_COLS])
        io["w2f"] = din("w2f", [17, 1024])
        io["w2b"] = din("w2b", [17, 1024])
        io["gnwT"] = din("gnwT", [128, 4])
        io["snwT"] = din("snwT", [128, 32])
        io["fnw"] = din("fnw", [1, D])
        io["conv_wT"] = din("conv_wT", [128, 48, 9])
        io["conv_bT"] = din("conv_bT", [128, 48])
        io["ssd_rows"] = din("ssd_rows", [1, 5 * 64])
        io["b_gateT"] = din("b_gateT", [128, 32])
        io["w_gla_out"] = din("w_gla_out", [D, D])
        io["w_ssd_out"] = din("w_ssd_out", [2 * D, D])
        io["w_o"] = din("w_o", [D, D])
        io["wq"] = din("wq", [D, D])
        io["keysT"] = din("keysT", [128, 16, 128])
        io["uT"] = din("uT", [D, 16384])
        io["pv"] = din("pv", [16384, D])
        io["consts"] = din("consts", [128, 8, 128])
        io["out"] = nc.dram_tensor("out", [nb, SEQ, D], F32, kind="ExternalOutput").ap()
        skind = "ExternalOutput" if self.dbg.get("scratch_out") else "Internal"
        io["s_og"] = nc.dram_tensor("s_og", [nb, SEQ, D], F32, kind=skind).ap()
        io["s_ys"] = nc.dram_tensor("s_ys", [nb, SEQ, 2 * D], F32, kind=skind).ap()
        io["s_x1"] = nc.dram_tensor("s_x1", [nb, SEQ, D], F32, kind=skind).ap()
        io["s_og2"] = nc.dram_tensor("s_og2", [nb, SEQ, D], F32, kind=skind).ap()
        io["s_ys2"] = nc.dram_tensor("s_ys2", [nb, SEQ, 2 * D], F32, kind=skind).ap()
        for name, shape in self.dbg.get("outs", {}).items():
            io[name] = nc.dram_tensor(name, list(shape), F32, kind="ExternalOutput").ap()
        self.io = io

        with ExitStack() as es:
            S = Sched(nc, es)
            self.S = S
            self.ps = [es.enter_context(nc.psum_tensor("ps%d" % i, [128, 512], F32)) for i in range(8)]
            self.ps_rot = list(range(8))
            self.ps_i = 0
            P = {}
            P["consts"] = self.sb(es, "consts", [128, 8, 128])
            P["cbf"] = self.sb(es, "cbf", [128, 8, 128], BF16)
            P["modT"] = self.sb(es, "modT", [128, 96, 3])
            P["A1"] = self.sb(es, "A1", [128, KC, 3])
            P["A2"] = self.sb(es, "A2", [128, KC, 3])
            P["ones_bf"] = self.sb(es, "ones_bf", [128, 128], BF16)
            self.P = P
            S.dma(lambda e: e.dma_start(out=P["consts"][:], in_=io["consts"]), writes=["consts"])
            S.op("dve", lambda e: e.tensor_copy(P["cbf"][:], P["consts"][:]), reads=["consts"], writes=["cbf"])
            S.op("dve", lambda e: e.memset(P["ones_bf"][:], 1.0), writes=["ones_bf"])

            self.precast()
            if "0" in self.stages:
                self.stage0()
            for b in range(nb):
                with ExitStack() as es_seq:
                    S.barrier()
                    self.hT = self.sb(es_seq, "hT", [128, KC, LTOT], BF16)
                    if "A" in self.stages:
                        self.stageA(b)
                    if "B" in self.stages:
                        self.stageB(b)
                    if "C" in self.stages:
                        self.stageC(b)
                    if "D" in self.stages:
                        self.stageD(b)
                S.barrier()
                if "E" in self.stages:
                    self.stageE(b)
            S.wait_all("sp")
            S.emit()
        return nc

    def cst(self, i):
        return self.P["consts"][:, i, :]

    def cstb(self, i):
        return self.P["cbf"][:, i, :]

    def stage0(self):
        nc, S, io, P = self.nc, self.S, self.io, self.P
        with ExitStack() as es:
            cT = self.sb(es, "cT", [128, KC, 3])
            sc = self.sb(es, "sc", [128, KC, 3])
            badT = self.sb(es, "badT", [128, 96])
            n1T = self.sb(es, "n1T", [128, KC])
            n2T = self.sb(es, "n2T", [128, KC])
            slab = [self.sb(es, "adaslab", [128, KC, 512]) for _ in range(2)]
            S.dma(lambda e: e.dma_start(out=cT[:], in_=io["cT"]), writes=["cT"])
            S.dma(lambda e: e.dma_start(out=badT[:], in_=io["b_adaT"]), writes=["badT"])
            S.dma(lambda e: e.dma_start(out=n1T[:], in_=io["n1T"]), writes=["n1T"])
            S.dma(lambda e: e.dma_start(out=n2T[:], in_=io["n2T"]), writes=["n2T"])
            S.op("act", lambda e: e.activation(out=sc[:], in_=cT[:], func=AF.Silu), reads=["cT"], writes=["sc"])
            wv = io["w_ada"].rearrange("(kc p) c -> p kc c", p=128)
            pb = self.ps_next()
            psm = self.ps[pb]
            for blk in range(24):
                sl = slab[blk % 2]
                key = "adaslab%d" % (blk % 2)
                S.dma(lambda e, sl=sl, blk=blk: e.dma_start(out=sl[:], in_=wv[:, :, blk * 512:(blk + 1) * 512]),
                      writes=[key])
                for cc in range(4):
                    c = blk * 4 + cc
                    for kc in range(KC):
                        S.op("pe", lambda e, sl=sl, cc=cc, kc=kc, c=c: e.matmul(
                            psm[:, c * 3:c * 3 + 3], sl[:, kc, cc * 128:(cc + 1) * 128], sc[:, kc, :],
                            start=(kc == 0), stop=(kc == KC - 1)),
                            reads=[key, "sc"], writes=[("ps", pb)])
            modT = P["modT"]
            S.op("dve", lambda e: e.tensor_tensor(
                out=modT[:], in0=psm[:, 0:288].rearrange("p (c j) -> p c j", j=3),
                in1=badT[:].unsqueeze(2).to_broadcast([128, 96, 3]), op=ALU.add),
                reads=[("ps", pb), "badT"], writes=["modT"])
            for (A, nT, key, c0, akey) in ((P["A1"], n1T, "n1T", 16, "A1"), (P["A2"], n2T, "n2T", 64, "A2")):
                S.op("dve", lambda e, A=A, nT=nT, c0=c0: e.scalar_tensor_tensor(
                    out=A[:], in0=modT[:, c0:c0 + KC, :], scalar=1.0,
                    in1=nT[:].unsqueeze(2).to_broadcast([128, KC, 3]), op0=ALU.add, op1=ALU.mult),
                    reads=["modT", key], writes=[akey])
            self.S.barrier()

    def mod_col(self, which, kc, j):
        return self.P["modT"][:, which * KC + kc, j:j + 1]

    def norm_to_T(self, es_tmp, src_key, src, A, SH_which, j, dst_fn, dst_keys, tmp):
        S = self.S
        xs, junk, ss = tmp["xs"], tmp["junk"], tmp["ss"]
        S.op("act", lambda e: e.activation(out=junk[:], in_=src, func=AF.Square, accum_out=ss[:, 0:1]),
             reads=[src_key], writes=["nt_junk", "nt_ss"])
        S.op("dve", lambda e: e.tensor_scalar(out=ss[:, 1:2], in0=ss[:, 0:1], scalar1=1.0 / D, scalar2=EPS,
                                               op0=ALU.mult, op1=ALU.add), reads=["nt_ss"], writes=["nt_ss1"])
        S.op("act", lambda e: e.activation(out=ss[:, 2:3], in_=ss[:, 1:2], func=AF.Sqrt), reads=["nt_ss1"], writes=["nt_ss2"])
        S.op("dve", lambda e: e.reciprocal(out=ss[:, 3:4], in_=ss[:, 2:3]), reads=["nt_ss2"], writes=["nt_rstd"])
        S.op("dve", lambda e: e.tensor_scalar(out=xs[:], in0=src, scalar1=ss[:, 3:4], scalar2=None, op0=ALU.mult),
             reads=[src_key, "nt_rstd"], writes=["nt_xs"])
        for g4 in range(4):
            pb = self.ps_next()
            for q in range(4):
                kc = g4 * 4 + q
                S.op("pe", lambda e, pb=pb, q=q, kc=kc: e.transpose(
                    self.ps[pb][:, q * 128:(q + 1) * 128], xs[:, kc * 128:(kc + 1) * 128], self.cst(0)),
                    reads=["nt_xs", "consts"], writes=[("ps", pb)])
            for q in range(4):
                kc = g4 * 4 + q
                S.op("dve", lambda e, pb=pb, q=q, kc=kc: e.tensor_scalar(
                    out=dst_fn(kc), in0=self.ps[pb][:, q * 128:(q + 1) * 128],
                    scalar1=A[:, kc, j:j + 1], scalar2=self.mod_col(SH_which, kc, j), op0=ALU.mult, op1=ALU.add),
                    reads=[("ps", pb), "modT"], writes=dst_keys)

    def stageA(self, b):
        S, io = self.S, self.io
        with ExitStack() as es:
            xt = [self.sb(es, "xt", [128, D]) for _ in range(2)]
            tmp = {"xs": self.sb(es, "xs", [128, D]), "junk": self.sb(es, "junk", [128, D]),
                   "ss": self.sb(es, "ss", [128, 4])}
            for t in range(NT):
                buf = xt[t % 2]
                key = "xt%d" % (t % 2)
                if t < NTC:
                    src = io["ctx"][b, t * 128:(t + 1) * 128, :]
                    j = 2
                else:
                    src = io["x"][b, (t - NTC) * 128:(t - NTC + 1) * 128, :]
                    j = b
                S.dma(lambda e, buf=buf, src=src: e.dma_start(out=buf[:], in_=src), writes=[key])
                self.norm_to_T(es, key, buf[:], self.P["A1"], 0, j,
                               lambda kc, t=t: self.hT[:, kc, t * 128:(t + 1) * 128], [("hT", t)], tmp)
            if "hT" in self.dbg.get("outs", {}) and b == 0:
                self.dump_bf16(es, self.hT[:, :, :], [128, KC, LTOT], "hT", [("hT", t) for t in range(NT)])
        S.barrier()

    def dump_bf16(self, es, ap, shape, name, keys):
        S = self.S
        t = self.sb(es, "dump", [128, shape[2]])
        for i in range(shape[1]):
            S.op("dve", lambda e, i=i: e.tensor_copy(t[:], ap[:, i, :]), reads=keys, writes=["dump"])
            S.dma(lambda e, i=i: e.dma_start(out=self.io[name][:, i, :], in_=t[:]), reads=["dump"], writes=["dbg_" + name])

    BIGW = ("w_in", "w_gla_out", "w_ssd_out", "w_o", "wq", "uT", "pv")

    def precast(self):
        nc, S, io = self.nc, self.S, self.io
        self.bf = {}
        self.bfkeys = {}
        for name in self.BIGW:
            src = io[name]
            rows, cols = src.shape
            dst = nc.dram_tensor(name + "_bf", [rows, cols], BF16, kind="Internal").ap()
            self.bf[id(src)] = (name, dst)
            step = 256 if cols > 8192 else 1024
            keys = []
            for r0 in range(0, rows, step):
                k = ("wbf", name, r0)
                keys.append(k)
                self.pc_i = getattr(self, "pc_i", 0) + 1
                S.dma(lambda e, dst=dst, src=src, r0=r0, step=step: e.dma_start(out=dst[r0:r0 + step, :], in_=src[r0:r0 + step, :]),
                      writes=[k, ("pcslot", self.pc_i % 3)], q="pool")
            self.bfkeys[name] = keys

    def wsrc(self, wap):
        name, dst = self.bf[id(wap)]
        return dst, self.bfkeys[name]

    def load_w(self, buf, key, wap, c0, ncols, nk=KC, r0=0, b0=0):
        dst, keys = self.wsrc(wap)
        wv = dst[r0:r0 + nk * 128, :].rearrange("(kc p) c -> p kc c", p=128)
        self.S.dma(lambda e: e.dma_start(out=buf[:, 0:nk, b0:b0 + ncols], in_=wv[:, :, c0:c0 + ncols]),
                   reads=keys, writes=[key], q="sp")

    def tok_blocks(self):
        return [(0, 512), (512, 512), (1024, 512), (1536, 512), (2048, 256)]

    def scan_order(self, d):
        if d == 0:
            return list(range(NT))
        return [1, 0] + list(range(NT - 1, NTC - 1, -1))

    def stageB(self, b):
        S, io, P, hT = self.S, self.io, self.P, self.hT
        with ExitStack() as es:
            wsl = [self.sb(es, "wsl", [128, KC, 512], BF16) for _ in range(2)]
            lrT = [self.sb(es, "lrT", [17, LTOT], BF16) for _ in range(2)]
            w2 = [self.sb(es, "w2", [17, 1024], BF16) for _ in range(2)]
            qT = self.sb(es, "qT", [128, 2, LTOT], BF16)
            kT = self.sb(es, "kT", [128, 2, LTOT], BF16)
            vv = self.sb(es, "vv", [128, NT, 512], BF16)
            ktm = self.sb(es, "ktm", [128, NT, 256], BF16)
            Sst_ = [self.sb(es, "Sst", [128, 2, 512]) for _ in range(2)]
            Sbf_ = [self.sb(es, "Sbf", [128, 2, 512], BF16) for _ in range(2)]
            e1_ = [self.sb(es, "e1", [128, 256]) for _ in range(2)]
            gneg_ = [self.sb(es, "gneg", [128, 256]) for _ in range(2)]
            eq_ = [self.sb(es, "eq", [128, 2, 128]) for _ in range(2)]
            ek_ = [self.sb(es, "ek", [128, 2, 128]) for _ in range(2)]
            ekt_ = [self.sb(es, "ekt", [128, 256]) for _ in range(2)]
            qd_ = [self.sb(es, "qd", [128, 2, 128], BF16) for _ in range(2)]
            kd_ = [self.sb(es, "kd", [128, 2, 128], BF16) for _ in range(2)]
            kt_ = [self.sb(es, "kt", [128, 256], BF16) for _ in range(2)]
            aTm_ = [self.sb(es, "aTm", [128, 128], BF16) for _ in range(2)]
            osb_ = [self.sb(es, "osb", [128, 512]) for _ in range(2)]
            for d, nm in ((0, "w2f"), (1, "w2b")):
                S.dma(lambda e, d=d, nm=nm: e.dma_start(out=w2[d][:], in_=io[nm]), writes=[("w2", d)], q="pool")
                S.op("dve", lambda e, d=d: e.memset(lrT[d][:], 1.0), writes=[("lrT", d)])
            self.load_w(wsl[0], ("wsl", 0), io["w_in"], C_LRF, 32)
            for d in range(2):
                for (t0, n) in self.tok_blocks():
                    pb = self.ps_next()
                    for kc in range(KC):
                        S.op("pe", lambda e, pb=pb, kc=kc, t0=t0, n=n, d=d: e.matmul(
                            self.ps[pb][0:16, 0:n], wsl[0][:, kc, d * 16:(d + 1) * 16], hT[:, kc, t0:t0 + n],
                            start=(kc == 0), stop=(kc == KC - 1)),
                            reads=[("wsl", 0)] + [("hT", t) for t in range(NT)], writes=[("ps", pb)])
                    S.op("act", lambda e, pb=pb, t0=t0, n=n, d=d: e.copy(lrT[d][0:16, t0:t0 + n], self.ps[pb][0:16, 0:n]),
                         reads=[("ps", pb)], writes=[("lrT", d)])
            hkeys = [("hT", t) for t in range(NT)]
            for h in self.dbg.get('gla_heads', range(4)):
                self.load_w(wsl[0], ("wsl", 0), io["w_in"], C_Q + h * 256, 256)
                self.load_w(wsl[0], ("wsl", 0, "b"), io["w_in"], C_K + h * 256, 256, b0=256)
                self.load_w(wsl[1], ("wsl", 1), io["w_in"], C_V + h * 512, 512)
                for cc in range(4):
                    for (t0, n) in self.tok_blocks():
                        pb = self.ps_next()
                        for kc in range(KC):
                            S.op("pe", lambda e, pb=pb, kc=kc, t0=t0, n=n, cc=cc: e.matmul(
                                self.ps[pb][:, 0:n], wsl[0][:, kc, cc * 128:(cc + 1) * 128], hT[:, kc, t0:t0 + n],
                                start=(kc == 0), stop=(kc == KC - 1)),
                                reads=[("wsl", 0), ("wsl", 0, "b")] + hkeys, writes=[("ps", pb)])
                        if cc < 2:
                            S.op("act", lambda e, pb=pb, t0=t0, n=n, cc=cc: e.activation(
                                out=qT[:, cc, t0:t0 + n], in_=self.ps[pb][:, 0:n], func=AF.Copy, scale=1.0 / 16.0),
                                reads=[("ps", pb)], writes=["qT"])
                        else:
                            S.op("dve", lambda e, pb=pb, t0=t0, n=n, cc=cc: e.tensor_copy(
                                kT[:, cc - 2, t0:t0 + n], self.ps[pb][:, 0:n]),
                                reads=[("ps", pb)], writes=["kT"])
                for t in range(NT):
                    pb = self.ps_next()
                    for kc in range(KC):
                        S.op("pe", lambda e, pb=pb, kc=kc, t=t: e.matmul(
                            self.ps[pb][:, :], hT[:, kc, t * 128:(t + 1) * 128], wsl[1][:, kc, :],
                            start=(kc == 0), stop=(kc == KC - 1)),
                            reads=[("wsl", 1), ("hT", t)], writes=[("ps", pb)])
                    S.op("act", lambda e, pb=pb, t=t: e.copy(vv[:, t, :], self.ps[pb][:, :]),
                         reads=[("ps", pb)], writes=[("vv", t)])
                    pb = self.ps_next()
                    for kc in range(KC):
                        S.op("pe", lambda e, pb=pb, kc=kc, t=t: e.matmul(
                            self.ps[pb][:, 0:256], hT[:, kc, t * 128:(t + 1) * 128], wsl[0][:, kc, 256:512],
                            start=(kc == 0), stop=(kc == KC - 1)),
                            reads=[("wsl", 0), ("wsl", 0, "b"), ("hT", t)], writes=[("ps", pb)])
                    S.op("dve", lambda e, pb=pb, t=t: e.tensor_copy(ktm[:, t, :], self.ps[pb][:, 0:256]),
                         reads=[("ps", pb)], writes=[("ktm", t)])
                for d in range(2):
                    S.op("dve", lambda e, d=d: e.memset(Sst_[d][:], 0.0), writes=[("Sst", d)])
                    S.op("dve", lambda e, d=d: e.memset(Sbf_[d][:], 0.0), writes=[("Sbf", d)])
                for step in range(NT):
                    for d in range(2):
                        t = self.scan_order(d)[step]
                        TI = self.cst(1 + d)
                        TS = self.cst(3 + d)
                        MK = self.cst(5 + d)
                        last = 127 if d == 0 else 0
                        Sst, Sbf, e1, gneg, eq, ek, ekt, osb = Sst_[d], Sbf_[d], e1_[d], gneg_[d], eq_[d], ek_[d], ekt_[d], osb_[d]
                        qd, kd, kt, aTm = qd_[d], kd_[d], kt_[d], aTm_[d]
                        isx = t >= NTC
                        tsl = slice(t * 128, (t + 1) * 128)
                        p_lg = self.ps_next()
                        S.op("pe", lambda e, Sst=Sst, Sbf=Sbf, e1=e1, gneg=gneg, eq=eq, ek=ek, ekt=ekt, osb=osb, qd=qd, kd=kd, kt=kt, aTm=aTm, p_lg=p_lg, tsl=tsl, d=d, h=h: e.matmul(
                            self.ps[p_lg][:, 0:256], lrT[d][0:17, tsl], w2[d][0:17, h * 256:(h + 1) * 256],
                            start=True, stop=True), reads=[("lrT", d), ("w2", d)], writes=[("ps", p_lg)])
                        S.op("act", lambda e, Sst=Sst, Sbf=Sbf, e1=e1, gneg=gneg, eq=eq, ek=ek, ekt=ekt, osb=osb, qd=qd, kd=kd, kt=kt, aTm=aTm, p_lg=p_lg: e.activation(out=e1[:], in_=self.ps[p_lg][:, 0:256], func=AF.Exp, scale=-1.0),
                             reads=[("ps", p_lg)], writes=[("e1", d)])
                        S.op("act", lambda e, Sst=Sst, Sbf=Sbf, e1=e1, gneg=gneg, eq=eq, ek=ek, ekt=ekt, osb=osb, qd=qd, kd=kd, kt=kt, aTm=aTm: e.activation(out=gneg[:], in_=e1[:], func=AF.Ln, bias=1.0),
                             reads=[("e1", d)], writes=[("gneg", d)])
                        p_bt = self.ps_next()
                        for cc in range(2):
                            S.op("pe", lambda e, Sst=Sst, Sbf=Sbf, e1=e1, gneg=gneg, eq=eq, ek=ek, ekt=ekt, osb=osb, qd=qd, kd=kd, kt=kt, aTm=aTm, p_bt=p_bt, cc=cc, TI=TI: e.matmul(
                                self.ps[p_bt][:, cc * 128:(cc + 1) * 128], gneg[:, cc * 128:(cc + 1) * 128], TI,
                                start=True, stop=True), reads=[("gneg", d), "consts"], writes=[("ps", p_bt)])
                        p_r = self.ps_next()
                        S.op("pe", lambda e, Sst=Sst, Sbf=Sbf, e1=e1, gneg=gneg, eq=eq, ek=ek, ekt=ekt, osb=osb, qd=qd, kd=kd, kt=kt, aTm=aTm, p_r=p_r, TS=TS: e.matmul(
                            self.ps[p_r][:, 0:256], TS, gneg[:], start=True, stop=True),
                            reads=[("gneg", d), "consts"], writes=[("ps", p_r)])
                        btv = self.ps[p_bt][:, 0:256].rearrange("p (c i) -> p c i", c=2)
                        S.op("act", lambda e, Sst=Sst, Sbf=Sbf, e1=e1, gneg=gneg, eq=eq, ek=ek, ekt=ekt, osb=osb, qd=qd, kd=kd, kt=kt, aTm=aTm, btv=btv: e.activation(out=eq[:], in_=btv, func=AF.Exp, scale=-1.0 / 16.0),
                             reads=[("ps", p_bt)], writes=[("eq", d)])
                        S.op("act", lambda e, Sst=Sst, Sbf=Sbf, e1=e1, gneg=gneg, eq=eq, ek=ek, ekt=ekt, osb=osb, qd=qd, kd=kd, kt=kt, aTm=aTm, btv=btv: e.activation(out=ek[:], in_=btv, func=AF.Exp, scale=1.0 / 16.0),
                             reads=[("ps", p_bt)], writes=[("ek", d)])
                        S.op("act", lambda e, Sst=Sst, Sbf=Sbf, e1=e1, gneg=gneg, eq=eq, ek=ek, ekt=ekt, osb=osb, qd=qd, kd=kd, kt=kt, aTm=aTm, p_r=p_r: e.activation(out=ekt[:], in_=self.ps[p_r][:, 0:256], func=AF.Exp, scale=-1.0 / 16.0),
                             reads=[("ps", p_r)], writes=[("ekt", d)])
                        S.op("dve", lambda e, Sst=Sst, Sbf=Sbf, e1=e1, gneg=gneg, eq=eq, ek=ek, ekt=ekt, osb=osb, qd=qd, kd=kd, kt=kt, aTm=aTm, tsl=tsl: e.tensor_tensor(out=kt[:], in0=ktm[:, tsl.start // 128, :], in1=ekt[:], op=ALU.mult),
                             reads=[("ktm", t), ("ekt", d)], writes=[("kt", d)])
                        if isx:
                            S.op("dve", lambda e, Sst=Sst, Sbf=Sbf, e1=e1, gneg=gneg, eq=eq, ek=ek, ekt=ekt, osb=osb, qd=qd, kd=kd, kt=kt, aTm=aTm, tsl=tsl: e.tensor_tensor(out=qd[:], in0=qT[:, :, tsl], in1=eq[:], op=ALU.mult),
                                 reads=["qT", ("eq", d)], writes=[("qd", d)])
                            S.op("dve", lambda e, Sst=Sst, Sbf=Sbf, e1=e1, gneg=gneg, eq=eq, ek=ek, ekt=ekt, osb=osb, qd=qd, kd=kd, kt=kt, aTm=aTm, tsl=tsl: e.tensor_tensor(out=kd[:], in0=kT[:, :, tsl], in1=ek[:], op=ALU.mult),
                                 reads=["kT", ("ek", d)], writes=[("kd", d)])
                            p_a = self.ps_next()
                            for cc in range(2):
                                S.op("pe", lambda e, Sst=Sst, Sbf=Sbf, e1=e1, gneg=gneg, eq=eq, ek=ek, ekt=ekt, osb=osb, qd=qd, kd=kd, kt=kt, aTm=aTm, p_a=p_a, cc=cc: e.matmul(
                                    self.ps[p_a][:, 0:128], kd[:, cc, :], qd[:, cc, :], start=(cc == 0), stop=(cc == 1)),
                                    reads=[("kd", d), ("qd", d)], writes=[("ps", p_a)])
                            S.op("dve", lambda e, Sst=Sst, Sbf=Sbf, e1=e1, gneg=gneg, eq=eq, ek=ek, ekt=ekt, osb=osb, qd=qd, kd=kd, kt=kt, aTm=aTm, p_a=p_a, MK=MK: e.tensor_tensor(out=aTm[:], in0=self.ps[p_a][:, 0:128], in1=MK, op=ALU.mult),
                                 reads=[("ps", p_a), "consts"], writes=[("aTm", d)])
                            p_o = self.ps_next()
                            S.op("pe", lambda e, Sst=Sst, Sbf=Sbf, e1=e1, gneg=gneg, eq=eq, ek=ek, ekt=ekt, osb=osb, qd=qd, kd=kd, kt=kt, aTm=aTm, p_o=p_o, t=t: e.matmul(self.ps[p_o][:, :], aTm[:], vv[:, t, :], start=True, stop=False),
                                 reads=[("aTm", d), ("vv", t)], writes=[("ps", p_o)])
                            for cc in range(2):
                                S.op("pe", lambda e, Sst=Sst, Sbf=Sbf, e1=e1, gneg=gneg, eq=eq, ek=ek, ekt=ekt, osb=osb, qd=qd, kd=kd, kt=kt, aTm=aTm, p_o=p_o, cc=cc: e.matmul(self.ps[p_o][:, :], qd[:, cc, :], Sbf[:, cc, :], start=False, stop=(cc == 1)),
                                     reads=[("qd", d), ("Sbf", d)], writes=[("ps", p_o)])
                            dst = io["s_og" if d == 0 else "s_og2"][b, (t - NTC) * 128:(t - NTC + 1) * 128, h * 512:(h + 1) * 512]
                            okey = ("s_og", d, t, h)
                            S.op("act", lambda e, Sst=Sst, Sbf=Sbf, e1=e1, gneg=gneg, eq=eq, ek=ek, ekt=ekt, osb=osb, qd=qd, kd=kd, kt=kt, aTm=aTm, p_o=p_o: e.copy(osb[:], self.ps[p_o][:, :]), reads=[("ps", p_o)], writes=[("osb", d)])
                            S.dma(lambda e, Sst=Sst, Sbf=Sbf, e1=e1, gneg=gneg, eq=eq, ek=ek, ekt=ekt, osb=osb, qd=qd, kd=kd, kt=kt, aTm=aTm, dst=dst: e.dma_start(out=dst, in_=osb[:]), reads=[("osb", d)], writes=[okey])
                        for cc in range(2):
                            p_s = self.ps_next()
                            S.op("pe", lambda e, Sst=Sst, Sbf=Sbf, e1=e1, gneg=gneg, eq=eq, ek=ek, ekt=ekt, osb=osb, qd=qd, kd=kd, kt=kt, aTm=aTm, p_s=p_s, cc=cc, t=t: e.matmul(
                                self.ps[p_s][:, :], kt[:, cc * 128:(cc + 1) * 128], vv[:, t, :], start=True, stop=True),
                                reads=[("kt", d), ("vv", t)], writes=[("ps", p_s)])
                            S.op("dve", lambda e, Sst=Sst, Sbf=Sbf, e1=e1, gneg=gneg, eq=eq, ek=ek, ekt=ekt, osb=osb, qd=qd, kd=kd, kt=kt, aTm=aTm, p_s=p_s, cc=cc, last=last: e.scalar_tensor_tensor(
                                out=Sst[:, cc, :], in0=Sst[:, cc, :], scalar=eq[:, cc, last:last + 1], in1=self.ps[p_s][:, :],
                                op0=ALU.mult, op1=ALU.add), reads=[("Sst", d), ("eq", d), ("ps", p_s)], writes=[("Sst", d)])
                        S.op("act", lambda e, Sst=Sst, Sbf=Sbf, e1=e1, gneg=gneg, eq=eq, ek=ek, ekt=ekt, osb=osb, qd=qd, kd=kd, kt=kt, aTm=aTm: e.copy(Sbf[:], Sst[:]), reads=[("Sst", d)], writes=[("Sbf", d)])
        S.barrier()

    def stageC(self, b):
        S, io, P, hT = self.S, self.io, self.P, self.hT
        hkeys = [("hT", t) for t in range(NT)]
        with ExitStack() as es:
            wsl = [self.sb(es, "wslc", [128, KC, 512], BF16) for _ in range(2)]
            rows = self.sb(es, "rows", [1, 320])
            rows_bf = self.sb(es, "rows_bf", [1, 128], BF16)
            negA = self.sb(es, "negA", [128, 128])
            dsk = self.sb(es, "dsk", [128, 64])
            dt_all = self.sb(es, "dt_all", [128, NT, 128])
            dta_all = self.sb(es, "dta_all", [128, NT, 128])
            cw = self.sb(es, "cw", [128, 48, 9])
            cb = self.sb(es, "cb", [128, 48])
            raw = self.sb(es, "raw", [128, LTOT])
            acc = self.sb(es, "acc", [128, LTOT])
            post = raw
            xs_tm = self.sb(es, "xs_tm", [128, NT, 512], BF16)
            Btm = self.sb(es, "Btm", [128, NT, 128], BF16)
            BT = self.sb(es, "BT", [128, LTOT], BF16)
            CT = self.sb(es, "CT", [128, LTOT], BF16)
            Sst = self.sb(es, "SstC", [128, 512])
            Sbf = self.sb(es, "SbfC", [128, 512], BF16)
            X = self.sb(es, "X", [128, 8, 128])
            cum = self.sb(es, "cum", [128, 8])
            seg = self.sb(es, "seg", [128, 8, 128])
            Lm = seg
            cbm = self.sb(es, "cbm", [128, 128])
            MT = self.sb(es, "MT", [128, 8, 128], BF16)
            xdt = self.sb(es, "xdt", [128, 8, 64], BF16)
            wj = self.sb(es, "wj", [128, 8, 64], BF16)
            ecum = self.sb(es, "ecum", [128, 8])
            ecl = self.sb(es, "ecl", [128, 8])
            wdec = self.sb(es, "wdec", [128, 8])
            t1 = self.sb(es, "t1", [128, 512])
            yo = self.sb(es, "yo", [128, 512])
            e1 = self.sb(es, "e1c", [128, 128])

            S.dma(lambda e: e.dma_start(out=rows[:], in_=io["ssd_rows"]), writes=["rows"])
            S.dma(lambda e: e.dma_start(out=cw[:], in_=io["conv_wT"]), writes=["cw"])
            S.dma(lambda e: e.dma_start(out=cb[:], in_=io["conv_bT"]), writes=["cb"])
            S.op("dve", lambda e: e.tensor_copy(rows_bf[:], rows[0:1, 192:320]), reads=["rows"], writes=["rows_bf"])
            pb = self.ps_next()
            S.op("pe", lambda e, pb=pb: e.matmul(self.ps[pb][:, 0:192], P["consts"][0:1, 7, :], rows[0:1, 0:192], start=True, stop=True),
                 reads=["consts", "rows"], writes=[("ps", pb)])
            S.op("act", lambda e, pb=pb: e.activation(out=negA[:], in_=self.ps[pb][:, 0:128], func=AF.Exp), reads=[("ps", pb)], writes=["negA"])
            S.op("dve", lambda e: e.tensor_scalar(out=negA[:], in0=negA[:], scalar1=-1.0, scalar2=None, op0=ALU.mult), reads=["negA"], writes=["negA"])
            S.op("dve", lambda e, pb=pb: e.tensor_copy(dsk[:], self.ps[pb][:, 128:192]), reads=[("ps", pb)], writes=["dsk"])
            self.load_w(wsl[0], ("wslc", 0), io["w_in"], C_DT, 128)
            for t in range(NT):
                pb = self.ps_next()
                for kc in range(KC):
                    S.op("pe", lambda e, pb=pb, kc=kc, t=t: e.matmul(
                        self.ps[pb][:, 0:128], hT[:, kc, t * 128:(t + 1) * 128], wsl[0][:, kc, 0:128],
                        start=(kc == 0), stop=False), reads=[("wslc", 0), ("hT", t)], writes=[("ps", pb)])
                S.op("pe", lambda e, pb=pb: e.matmul(self.ps[pb][:, 0:128], P["ones_bf"][0:1, :], rows_bf[0:1, :], start=False, stop=True),
                     reads=["ones_bf", "rows_bf"], writes=[("ps", pb)])
                S.op("act", lambda e, pb=pb: e.activation(out=e1[:], in_=self.ps[pb][:, 0:128], func=AF.Exp), reads=[("ps", pb)], writes=["e1c"])
                S.op("act", lambda e, t=t: e.activation(out=dt_all[:, t, :], in_=e1[:], func=AF.Ln, bias=1.0), reads=["e1c"], writes=["dt_all"])
                S.op("dve", lambda e, t=t: e.tensor_tensor(out=dta_all[:, t, :], in0=dt_all[:, t, :], in1=negA[:], op=ALU.mult),
                     reads=["dt_all", "negA"], writes=["dta_all"])

            for g in self.dbg.get("ssd_groups", range(8)):
                self.ps_rot = list(range(8))
                self.load_w(wsl[0], ("wslc", 0), io["w_in"], C_XS + g * 512, 512)
                self.load_w(wsl[1], ("wslc", 1), io["w_in"], C_B + g * 128, 128)
                self.load_w(wsl[1], ("wslc", 1, "b"), io["w_in"], C_C + g * 128, 128, b0=128)
                for ch in range(6):
                    if ch < 4:
                        wb, wk, c0, gch = wsl[0], ("wslc", 0), ch * 128, (C_XS - C_XS) // 128 + g * 4 + ch
                    elif ch == 4:
                        wb, wk, c0, gch = wsl[1], ("wslc", 1), 0, 32 + g
                    else:
                        wb, wk, c0, gch = wsl[1], ("wslc", 1), 128, 40 + g
                    for (t0, n) in self.tok_blocks():
                        pb = self.ps_next()
                        for kc in range(KC):
                            S.op("pe", lambda e, pb=pb, kc=kc, t0=t0, n=n, wb=wb, c0=c0: e.matmul(
                                self.ps[pb][:, 0:n], wb[:, kc, c0:c0 + 128], hT[:, kc, t0:t0 + n],
                                start=(kc == 0), stop=(kc == KC - 1)), reads=[wk, ("wslc", 1, "b")] + hkeys, writes=[("ps", pb)])
                        S.op("act", lambda e, pb=pb, t0=t0, n=n: e.copy(raw[:, t0:t0 + n], self.ps[pb][:, 0:n]),
                             reads=[("ps", pb)], writes=["raw"])
                    S.op("dve", lambda e, gch=gch: e.tensor_scalar(out=acc[:], in0=raw[:], scalar1=cw[:, gch, 4:5], scalar2=cb[:, gch:gch + 1],
                                                                      op0=ALU.mult, op1=ALU.add), reads=["raw", "cw", "cb"], writes=["acc"])
                    for dx in (-1, 1):
                        o0, o1 = max(0, -dx), CTXL - max(0, dx)
                        S.op("dve", lambda e, gch=gch, dx=dx, o0=o0, o1=o1: e.scalar_tensor_tensor(
                            out=acc[:, o0:o1], in0=raw[:, o0 + dx:o1 + dx], scalar=cw[:, gch, 4 + dx:5 + dx], in1=acc[:, o0:o1],
                            op0=ALU.mult, op1=ALU.add), reads=["raw", "cw", "acc"], writes=["acc"])
                    rawx = raw[:, CTXL:LTOT].rearrange("p (r c) -> p r c", c=64)
                    accx = acc[:, CTXL:LTOT].rearrange("p (r c) -> p r c", c=64)
                    for dy in (-1, 0, 1):
                        for dx in (-1, 0, 1):
                            if dy == 0 and dx == 0:
                                continue
                            r0, r1 = max(0, -dy), 32 - max(0, dy)
                            c0_, c1_ = max(0, -dx), 64 - max(0, dx)
                            tap = (dy + 1) * 3 + (dx + 1)
                            S.op("dve", lambda e, gch=gch, dy=dy, dx=dx, r0=r0, r1=r1, c0_=c0_, c1_=c1_, tap=tap: e.scalar_tensor_tensor(
                                out=accx[:, r0:r1, c0_:c1_], in0=rawx[:, r0 + dy:r1 + dy, c0_ + dx:c1_ + dx],
                                scalar=cw[:, gch, tap:tap + 1], in1=accx[:, r0:r1, c0_:c1_],
                                op0=ALU.mult, op1=ALU.add), reads=["raw", "cw", "acc"], writes=["acc"])
                    if ch < 5:
                        S.op("act", lambda e: e.activation(out=post[:], in_=acc[:], func=AF.Silu), reads=["acc", "raw"], writes=["raw"])
                        if ch == 4:
                            S.op("dve", lambda e: e.tensor_copy(BT[:], post[:]), reads=["raw"], writes=["BT"])
                        for t in range(NT):
                            pb = self.ps_next()
                            S.op("pe", lambda e, pb=pb, t=t: e.transpose(self.ps[pb][:, 0:128], post[:, t * 128:(t + 1) * 128], self.cst(0)),
                                 reads=["raw", "consts"], writes=[("ps", pb)])
                            if ch < 4:
                                S.op("act", lambda e, pb=pb, t=t, ch=ch: e.copy(xs_tm[:, t, ch * 128:(ch + 1) * 128], self.ps[pb][:, 0:128]),
                                     reads=[("ps", pb)], writes=[("xs_tm", t)])
                            else:
                                S.op("act", lambda e, pb=pb, t=t: e.copy(Btm[:, t, :], self.ps[pb][:, 0:128]),
                                     reads=[("ps", pb)], writes=[("Btm", t)])
                    else:
                        S.op("act", lambda e: e.activation(out=CT[:], in_=acc[:], func=AF.Silu), reads=["acc"], writes=["CT"])
                self.ps_rot = [4, 5, 6, 7]
                for d in self.dbg.get("ssd_dirs", range(2)):
                    TI = self.cst(1 + d)
                    MK = self.cst(5 + d)
                    last = 127 if d == 0 else 0
                    hc0 = d * 64 + g * 8
                    S.op("dve", lambda e: e.memset(Sst[:], 0.0), writes=["SstC"])
                    S.op("dve", lambda e: e.memset(Sbf[:], 0.0), writes=["SbfC"])
                    for t in self.scan_order(d):
                        isx = t >= NTC
                        tsl = slice(t * 128, (t + 1) * 128)
                        dta = dta_all[:, t, hc0:hc0 + 8]
                        dtv = dt_all[:, t, hc0:hc0 + 8]
                        S.op("pool", lambda e, dta=dta, TI=TI: e.tensor_tensor(
                            out=X[:], in0=dta.unsqueeze(2).to_broadcast([128, 8, 128]), in1=TI.unsqueeze(1).to_broadcast([128, 8, 128]), op=ALU.mult),
                            reads=["dta_all", "consts"], writes=["X"])
                        self.c_step = getattr(self, "c_step", 0) + 1
                        p_rb = [2 * (self.c_step % 2), 2 * (self.c_step % 2) + 1]
                        for hb in range(2):
                            S.op("pe", lambda e, hb=hb, p_rb=p_rb: e.matmul(
                                self.ps[p_rb[hb]][:, :], self.cst(7), X[:, hb * 4:(hb + 1) * 4, :], start=True, stop=True),
                                reads=["X", "consts"], writes=[("ps", p_rb[hb])])
                        p_cum = self.ps_next()
                        S.op("pe", lambda e, p_cum=p_cum, dta=dta, TI=TI: e.matmul(self.ps[p_cum][:, 0:8], TI, dta, start=True, stop=True),
                             reads=["dta_all", "consts"], writes=[("ps", p_cum)])
                        S.op("dve", lambda e, p_cum=p_cum: e.tensor_copy(cum[:], self.ps[p_cum][:, 0:8]), reads=[("ps", p_cum)], writes=["cum"])
                        rbv = [self.ps[p_rb[hb]][:, :].rearrange("p (h i) -> p h i", h=4) for hb in range(2)]
                        xsv = xs_tm[:, t, :].rearrange("p (h q) -> p h q", h=8)
                        S.op("pool", lambda e, xsv=xsv, dtv=dtv: e.tensor_tensor(out=xdt[:], in0=xsv, in1=dtv.unsqueeze(2).to_broadcast([128, 8, 64]), op=ALU.mult),
                             reads=[("xs_tm", t), "dt_all"], writes=["xdt"])
                        if isx:
                            p_ys = self.ps_next()
                            S.op("pe", lambda e, p_ys=p_ys, tsl=tsl: e.matmul(self.ps[p_ys][:, :], CT[:, tsl], Sbf[:], start=True, stop=True),
                                 reads=["CT", "SbfC"], writes=[("ps", p_ys)])
                        for hb in range(2):
                            S.op("act", lambda e, hb=hb, rbv=rbv, last=last: e.activation(out=ecl[:, hb * 4:(hb + 1) * 4], in_=rbv[hb][:, :, last], func=AF.Exp),
                                 reads=[("ps", p_rb[hb])], writes=["ecl"])
                            S.op("dve", lambda e, hb=hb, rbv=rbv, last=last: e.tensor_tensor(out=wdec[:, hb * 4:(hb + 1) * 4], in0=rbv[hb][:, :, last],
                                                                                 in1=cum[:, hb * 4:(hb + 1) * 4], op=ALU.subtract),
                                 reads=[("ps", p_rb[hb]), "cum"], writes=["wdec"])
                        S.op("act", lambda e: e.activation(out=wdec[:], in_=wdec[:], func=AF.Exp), reads=["wdec"], writes=["wdec"])
                        S.op("pool", lambda e: e.tensor_tensor(out=wj[:], in0=xdt[:], in1=wdec[:].unsqueeze(2).to_broadcast([128, 8, 64]), op=ALU.mult),
                             reads=["xdt", "wdec"], writes=["wj"])
                        p_su = self.ps_next()
                        S.op("pe", lambda e, p_su=p_su, t=t: e.matmul(self.ps[p_su][:, :], Btm[:, t, :], wj[:].rearrange("p h q -> p (h q)"), start=True, stop=True),
                             reads=[("Btm", t), "wj"], writes=[("ps", p_su)])
                        S.op("dve", lambda e: e.tensor_tensor(out=Sst[:].rearrange("p (h q) -> p h q", h=8), in0=Sst[:].rearrange("p (h q) -> p h q", h=8),
                                                              in1=ecl[:].unsqueeze(2).to_broadcast([128, 8, 64]), op=ALU.mult),
                             reads=["SstC", "ecl"], writes=["SstC"])
                        S.op("dve", lambda e, p_su=p_su: e.tensor_tensor(out=Sst[:], in0=Sst[:], in1=self.ps[p_su][:, :], op=ALU.add),
                             reads=["SstC", ("ps", p_su)], writes=["SstC"])
                        S.op("act", lambda e: e.copy(Sbf[:], Sst[:]), reads=["SstC"], writes=["SbfC"])
                        if isx:
                            for hb in range(2):
                                S.op("dve", lambda e, hb=hb, rbv=rbv: e.tensor_tensor(
                                    out=seg[:, hb * 4:(hb + 1) * 4, :], in0=rbv[hb],
                                    in1=cum[:, hb * 4:(hb + 1) * 4].unsqueeze(2).to_broadcast([128, 4, 128]), op=ALU.subtract),
                                    reads=[("ps", p_rb[hb]), "cum"], writes=["seg"])
                            S.op("dve", lambda e: e.tensor_scalar(out=seg[:], in0=seg[:], scalar1=0.0, scalar2=None, op0=ALU.min),
                                 reads=["seg"], writes=["seg"])
                            S.op("act", lambda e: e.activation(out=Lm[:], in_=seg[:], func=AF.Exp), reads=["seg"], writes=["seg"])
                            p_cb = self.ps_next()
                            S.op("pe", lambda e, p_cb=p_cb, tsl=tsl: e.matmul(self.ps[p_cb][:, 0:128], BT[:, tsl], CT[:, tsl], start=True, stop=True),
                                 reads=["BT", "CT"], writes=[("ps", p_cb)])
                            S.op("dve", lambda e, p_cb=p_cb, MK=MK: e.tensor_tensor(out=cbm[:], in0=self.ps[p_cb][:, 0:128], in1=MK, op=ALU.mult),
                                 reads=[("ps", p_cb), "consts"], writes=["cbm"])
                            S.op("dve", lambda e: e.tensor_tensor(out=MT[:], in0=Lm[:], in1=cbm[:].unsqueeze(1).to_broadcast([128, 8, 128]), op=ALU.mult),
                                 reads=["seg", "cbm"], writes=["MT"])
                        if isx:
                            p_y = self.ps_next()
                            for hh in range(8):
                                S.op("pe", lambda e, p_y=p_y, hh=hh: e.matmul(self.ps[p_y][:, hh * 64:(hh + 1) * 64], MT[:, hh, :], xdt[:, hh, :], start=True, stop=True),
                                     reads=["MT", "xdt"], writes=[("ps", p_y)])
                            S.op("act", lambda e: e.activation(out=ecum[:], in_=cum[:], func=AF.Exp), reads=["cum"], writes=["ecum"])
                            S.op("dve", lambda e, p_ys=p_ys: e.tensor_tensor(
                                out=t1[:].rearrange("p (h q) -> p h q", h=8), in0=self.ps[p_ys][:, :].rearrange("p (h q) -> p h q", h=8),
                                in1=ecum[:].unsqueeze(2).to_broadcast([128, 8, 64]), op=ALU.mult), reads=[("ps", p_ys), "ecum"], writes=["t1"])
                            S.op("dve", lambda e, p_y=p_y: e.tensor_tensor(out=yo[:], in0=self.ps[p_y][:, :], in1=t1[:], op=ALU.add),
                                 reads=[("ps", p_y), "t1"], writes=["yo"])
                            dst = io["s_ys" if d == 0 else "s_ys2"][b, (t - NTC) * 128:(t - NTC + 1) * 128, g * 512:(g + 1) * 512]
                            ykey = ("s_ys", d, t, g)
                            if d == 0:
                                S.op("dve", lambda e, xsv=xsv, g=g: e.tensor_tensor(
                                    out=t1[:].rearrange("p (h q) -> p h q", h=8), in0=xsv,
                                    in1=dsk[:, g * 8:(g + 1) * 8].unsqueeze(2).to_broadcast([128, 8, 64]), op=ALU.mult),
                                    reads=[("xs_tm", t), "dsk"], writes=["t1"])
                                S.op("dve", lambda e: e.tensor_tensor(out=yo[:], in0=yo[:], in1=t1[:], op=ALU.add), reads=["yo", "t1"], writes=["yo"])
                            S.dma(lambda e, dst=dst: e.dma_start(out=dst, in_=yo[:]), reads=["yo"], writes=[ykey])
        self.ps_rot = list(range(8))
        S.barrier()

    def rstd_from_ss(self, ss, n, key, width):
        S = self.S
        S.op("dve", lambda e: e.tensor_scalar(out=ss[:, 0:width], in0=ss[:, 0:width], scalar1=1.0 / n, scalar2=EPS, op0=ALU.mult, op1=ALU.add),
             reads=[key], writes=[key])
        S.op("act", lambda e: e.activation(out=ss[:, 0:width], in_=ss[:, 0:width], func=AF.Sqrt), reads=[key], writes=[key])
        S.op("dve", lambda e: e.reciprocal(out=ss[:, 0:width], in_=ss[:, 0:width]), reads=[key], writes=[key])

    def row_bcast(self, dst, dkey, row_ap, rkey, n):
        S = self.S
        for c0 in range(0, n, 512):
            w = min(512, n - c0)
            pb = self.ps_next()
            S.op("pe", lambda e, pb=pb, c0=c0, w=w: e.matmul(self.ps[pb][:, 0:w], self.P["consts"][0:1, 7, :], row_ap[0:1, c0:c0 + w], start=True, stop=True),
                 reads=["consts", rkey], writes=[("ps", pb)])
            S.op("act", lambda e, pb=pb, c0=c0, w=w: e.copy(dst[:, c0:c0 + w], self.ps[pb][:, 0:w]), reads=[("ps", pb)], writes=[dkey])

    def col_to_bcast(self, dst, dkey, which, j, tmp):
        S = self.S
        modT = self.P["modT"]
        S.op("dve", lambda e: e.tensor_tensor(
            out=tmp[:].rearrange("p (k q) -> p k q", k=KC),
            in0=modT[:, which * KC:(which + 1) * KC, j:j + 1].to_broadcast([128, KC, 128]),
            in1=self.cst(0).unsqueeze(1).to_broadcast([128, KC, 128]), op=ALU.mult),
            reads=["modT", "consts"], writes=["c2b_tmp"])
        for c in range(4):
            pb = self.ps_next()
            S.op("pe", lambda e, pb=pb, c=c: e.matmul(self.ps[pb][:, :], self.cst(7), tmp[:, c * 512:(c + 1) * 512], start=True, stop=True),
                 reads=["c2b_tmp", "consts"], writes=[("ps", pb)])
            S.op("act", lambda e, pb=pb, c=c: e.copy(dst[:, c * 512:(c + 1) * 512], self.ps[pb][:, :]), reads=[("ps", pb)], writes=[dkey])

    TB = 2

    def stageD(self, b):
        S, io, P, hT = self.S, self.io, self.P, self.hT
        TB = self.TB
        n = TB * 128
        wv_in = io["w_in"]
        with ExitStack() as es:
            slA = self.sb(es, "slA", [128, KC, 512], BF16)
            slB = self.sb(es, "slB", [128, 32, 512], BF16)
            slG = [self.sb(es, "slG", [128, KC, 512], BF16) for _ in range(1)]
            oT = self.sb(es, "oT", [128, KC, n], BF16)
            yT = self.sb(es, "yT", [128, 32, n], BF16)
            mixT = self.sb(es, "mixT", [128, KC, n], BF16)
            gnwT = self.sb(es, "gnwT", [128, 4])
            snwT = self.sb(es, "snwT", [128, 32])
            bgT = self.sb(es, "bgT", [128, 32])
            g1bc = self.sb(es, "g1bc", [128, D])
            m1b = self.sb(es, "m1b", [128, 4, n])
            rs = self.sb(es, "rs", [128, 512])
            oh = self.sb(es, "oh", [128, 512])
            junk = self.sb(es, "junkd", [128, 512])
            onb = self.sb(es, "onb", [128, 512], BF16)
            ss = self.sb(es, "ssd", [128, 1])
            sa = self.sb(es, "sa", [128, n])
            sb_ = self.sb(es, "sb_", [128, n])
            m1 = self.sb(es, "m1", [128, n])
            m2 = self.sb(es, "m2", [128, n])
            S.dma(lambda e: e.dma_start(out=bgT[:], in_=io["b_gateT"]), writes=["bgT"])
            with ExitStack() as es2:
                tmpbig = self.sb(es2, "tmpbig", [128, D])
                S.dma(lambda e: e.dma_start(out=gnwT[:], in_=io["gnwT"]), writes=["gnwT"])
                S.dma(lambda e: e.dma_start(out=snwT[:], in_=io["snwT"]), writes=["snwT"])
                self.col_to_bcast(g1bc, "g1bc", 2, b, tmpbig)
                S.barrier()
            ptb = self.ps[7][:, :].bitcast(BF16)
            for blk in range(SEQ // n):
                tiles = [NTC + blk * TB + i for i in range(TB)]
                wbufs = [(slA, "slA"), (slG[0], ("slG", 0))]

                def d1_col(part):
                    return (C_R + part * 512) if part < 4 else (C_Z + (part - 4) * 512)

                self.load_w(wbufs[0][0], wbufs[0][1], wv_in, d1_col(0), 512)
                for part in range(12):
                    isg = part < 4
                    cur, curk = wbufs[part % 2]
                    if part + 1 < 12:
                        self.load_w(wbufs[(part + 1) % 2][0], wbufs[(part + 1) % 2][1], wv_in, d1_col(part + 1), 512)
                    for i, t in enumerate(tiles):
                        pb = self.ps_next()
                        for kc in range(KC):
                            S.op("pe", lambda e, pb=pb, kc=kc, t=t, cur=cur: e.matmul(self.ps[pb][:, :], hT[:, kc, t * 128:(t + 1) * 128], cur[:, kc, :],
                                                                                        start=(kc == 0), stop=(kc == KC - 1)),
                                 reads=[curk, ("hT", t)], writes=[("ps", pb)])
                        S.op("act", lambda e, pb=pb: e.activation(out=rs[:], in_=self.ps[pb][:, :], func=AF.Silu), reads=[("ps", pb)], writes=["rs"])
                        tok = slice((t - NTC) * 128, (t - NTC + 1) * 128)
                        if isg:
                            src = io["s_og"][b, tok, part * 512:(part + 1) * 512]
                            src2 = io["s_og2"][b, tok, part * 512:(part + 1) * 512]
                        else:
                            src = io["s_ys"][b, tok, (part - 4) * 512:(part - 3) * 512]
                            src2 = io["s_ys2"][b, tok, (part - 4) * 512:(part - 3) * 512]
                        S.dma(lambda e, src=src: e.dma_start(out=oh[:], in_=src), writes=["oh"])
                        S.dma(lambda e, src2=src2: e.dma_start(out=junk[:], in_=src2), writes=["junkd"])
                        S.op("dve", lambda e: e.tensor_tensor(out=oh[:], in0=oh[:], in1=junk[:], op=ALU.add), reads=["oh", "junkd"], writes=["oh"])
                        if isg:
                            S.op("act", lambda e: e.activation(out=junk[:], in_=oh[:], func=AF.Square, accum_out=ss[:, 0:1]), reads=["oh"], writes=["junkd", "ssd"])
                            self.rstd_from_ss(ss, 512, "ssd", 1)
                            S.op("dve", lambda e: e.scalar_tensor_tensor(out=onb[:], in0=oh[:], scalar=ss[:, 0:1], in1=rs[:], op0=ALU.mult, op1=ALU.mult),
                                 reads=["oh", "ssd", "rs"], writes=["onb"])
                        else:
                            gg = part - 4
                            S.op("dve", lambda e: e.tensor_tensor(out=oh[:], in0=oh[:], in1=rs[:], op=ALU.mult), reads=["oh", "rs"], writes=["oh"])
                            S.op("act", lambda e: e.activation(out=junk[:], in_=oh[:], func=AF.Square, accum_out=ss[:, 0:1]), reads=["oh"], writes=["junkd", "ssd"])
                            self.rstd_from_ss(ss, 512, "ssd", 1)
                            S.op("dve", lambda e: e.tensor_scalar(out=onb[:], in0=oh[:], scalar1=ss[:, 0:1], scalar2=None, op0=ALU.mult),
                                 reads=["oh", "ssd"], writes=["onb"])
                        for q in range(4):
                            S.op("pe", lambda e, q=q: e.transpose(ptb[:, q * 128:(q + 1) * 128], onb[:, q * 128:(q + 1) * 128], self.cstb(0)),
                                 reads=["onb", "cbf"], writes=[("ps", 7)])
                        for q in range(4):
                            if isg:
                                dstT, dk, wcol, wk = oT[:, part * 4 + q, i * 128:(i + 1) * 128], "oT", gnwT[:, q:q + 1], "gnwT"
                            else:
                                kk = (part - 4) * 4 + q
                                dstT, dk, wcol, wk = yT[:, kk, i * 128:(i + 1) * 128], "yT", snwT[:, kk:kk + 1], "snwT"
                            S.op("dve", lambda e, dstT=dstT, wcol=wcol, q=q: e.tensor_scalar(out=dstT, in0=ptb[:, q * 128:(q + 1) * 128], scalar1=wcol, scalar2=None, op0=ALU.mult),
                                 reads=[("ps", 7), wk], writes=[dk])
                hsl = slice(tiles[0] * 128, (tiles[-1] + 1) * 128)
                hk = [("hT", t) for t in tiles]
                self.load_w(slA, "slA", io["w_gla_out"], 0, 512)
                self.load_w(slG[0], ("slG", 0), wv_in, C_GL, 512)
                for s4 in range(4):
                    self.load_w(slB, "slB", io["w_ssd_out"], s4 * 512, 512, nk=32)
                    for q in range(4):
                        cc = s4 * 4 + q
                        cs = slice(q * 128, (q + 1) * 128)
                        p_ya, p_ga = self.ps_next(), self.ps_next()
                        for kc in range(KC):
                            S.op("pe", lambda e, kc=kc, cs=cs, p=p_ya: e.matmul(self.ps[p][:, 0:n], slA[:, kc, cs], oT[:, kc, :], start=(kc == 0), stop=(kc == KC - 1)),
                                 reads=["slA", "oT"], writes=[("ps", p_ya)])
                        for kc in range(KC):
                            S.op("pe", lambda e, kc=kc, cs=cs, p=p_ga, hsl=hsl: e.matmul(self.ps[p][:, 0:n], slG[0][:, kc, cs], hT[:, kc, hsl], start=(kc == 0), stop=(kc == KC - 1)),
                                 reads=[("slG", 0)] + hk, writes=[("ps", p_ga)])
                        S.op("act", lambda e, cc=cc, p=p_ga: e.activation(out=sa[:], in_=self.ps[p][:, 0:n], func=AF.Sigmoid, bias=bgT[:, cc:cc + 1]),
                             reads=[("ps", p_ga), "bgT"], writes=["sa"])
                        S.op("dve", lambda e, p=p_ya, q=q: e.tensor_tensor(out=m1b[:, q, :], in0=self.ps[p][:, 0:n], in1=sa[:], op=ALU.mult),
                             reads=[("ps", p_ya), "sa"], writes=["m1b"])
                    self.load_w(slG[0], ("slG", 0), wv_in, C_GL + D + s4 * 512, 512)
                    if s4 + 1 < 4:
                        self.load_w(slA, "slA", io["w_gla_out"], (s4 + 1) * 512, 512)
                    else:
                        self.load_w(slA, "slA", io["w_o"], 0, 512)
                    for q in range(4):
                        cc = s4 * 4 + q
                        cs = slice(q * 128, (q + 1) * 128)
                        p_yb, p_gb = self.ps_next(), self.ps_next()
                        for kc in range(32):
                            S.op("pe", lambda e, kc=kc, cs=cs, p=p_yb: e.matmul(self.ps[p][:, 0:n], slB[:, kc, cs], yT[:, kc, :], start=(kc == 0), stop=(kc == 31)),
                                 reads=["slB", "yT"], writes=[("ps", p_yb)])
                        for kc in range(KC):
                            S.op("pe", lambda e, kc=kc, cs=cs, p=p_gb, hsl=hsl: e.matmul(self.ps[p][:, 0:n], slG[0][:, kc, cs], hT[:, kc, hsl], start=(kc == 0), stop=(kc == KC - 1)),
                                 reads=[("slG", 0)] + hk, writes=[("ps", p_gb)])
                        S.op("act", lambda e, cc=cc, p=p_gb: e.activation(out=sb_[:], in_=self.ps[p][:, 0:n], func=AF.Sigmoid, bias=bgT[:, KC + cc:KC + cc + 1]),
                             reads=[("ps", p_gb), "bgT"], writes=["sb_"])
                        S.op("dve", lambda e, p=p_yb: e.tensor_tensor(out=m2[:], in0=self.ps[p][:, 0:n], in1=sb_[:], op=ALU.mult), reads=[("ps", p_yb), "sb_"], writes=["m2"])
                        S.op("dve", lambda e, cc=cc, q=q: e.tensor_tensor(out=mixT[:, cc, :], in0=m1b[:, q, :], in1=m2[:], op=ALU.add), reads=["m1b", "m2"], writes=["mixT"])
                    if s4 + 1 < 4:
                        self.load_w(slG[0], ("slG", 0), wv_in, C_GL + (s4 + 1) * 512, 512)
                    else:
                        self.load_w(slG[0], ("slG", 0), io["w_o"], 512, 512)
                for s4 in range(4):
                    cur, curk = wbufs[s4 % 2]
                    for i, t in enumerate(tiles):
                        tok = slice((t - NTC) * 128, (t - NTC + 1) * 128)
                        S.dma(lambda e, tok=tok, s4=s4: e.dma_start(out=oh[:], in_=io["x"][b, tok, s4 * 512:(s4 + 1) * 512]), writes=["oh"])
                        pb = self.ps_next()
                        for kc in range(KC):
                            S.op("pe", lambda e, pb=pb, kc=kc, i=i, cur=cur: e.matmul(self.ps[pb][:, :], mixT[:, kc, i * 128:(i + 1) * 128], cur[:, kc, :],
                                                                                        start=(kc == 0), stop=(kc == KC - 1)),
                                 reads=[curk, "mixT"], writes=[("ps", pb)])
                        S.op("dve", lambda e, pb=pb, s4=s4: e.tensor_tensor(out=rs[:], in0=self.ps[pb][:, :], in1=g1bc[:, s4 * 512:(s4 + 1) * 512], op=ALU.mult),
                             reads=[("ps", pb), "g1bc"], writes=["rs"])
                        S.op("dve", lambda e: e.tensor_tensor(out=junk[:], in0=oh[:], in1=rs[:], op=ALU.add), reads=["oh", "rs"], writes=["junkd"])
                        S.dma(lambda e, tok=tok, s4=s4: e.dma_start(out=io["s_x1"][b, tok, s4 * 512:(s4 + 1) * 512], in_=junk[:]),
                              reads=["junkd"], writes=[("s_x1", b, t, s4)])
                    if s4 + 2 < 4:
                        self.load_w(cur, curk, io["w_o"], (s4 + 2) * 512, 512)
        S.barrier()

    def stageE(self, b):
        S, io, P = self.S, self.io, self.P
        TB = self.TB
        n = TB * 128
        NEG = -1.0e30
        EW = 512
        NEB = 16384 // EW
        with ExitStack() as es:
            fnw_bc = self.sb(es, "fnw_bc", [128, D])
            g2bc = self.sb(es, "g2bc", [128, D])
            with ExitStack() as es2:
                fnw_r = self.sb(es2, "fnw_r", [1, D])
                tmpbig = self.sb(es2, "tmpbigE", [128, D])
                S.dma(lambda e: e.dma_start(out=fnw_r[:], in_=io["fnw"]), writes=["fnw_r"])
                self.row_bcast(fnw_bc, "fnw_bc", fnw_r, "fnw_r", D)
                self.col_to_bcast(g2bc, "g2bc", 5, b, tmpbig)
                S.barrier()
            slU = [self.sb(es, "slU", [128, KC, EW], BF16) for _ in range(2)]
            slV = [self.sb(es, "slV", [128, 4, D], BF16) for _ in range(2)]
            keysT = self.sb(es, "keysT", [128, 16, 128])
            x1b = self.sb(es, "x1e", [128, D])
            x1 = [x1b for _ in range(TB)]
            yacc = [self.sb(es, "yacc", [128, D]) for _ in range(TB)]
            hx2T = self.sb(es, "hx2T", [128, KC, n], BF16)
            qTp = self.sb(es, "qTp", [128, 16, n])
            sc = [self.sb(es, "sc", [128, 16, 128]) for _ in range(TB)]
            thr = [self.sb(es, "thr", [128, 8]) for _ in range(TB)]
            c0t = [self.sb(es, "c0t", [128, 8]) for _ in range(TB)]
            sv = self.sb(es, "sv", [128, 16, 16])
            tmp128 = self.sb(es, "tmp128", [128, 128])
            cand = self.sb(es, "cand", [128, 256])
            cand2 = self.sb(es, "cand2", [128, 256])
            cv = self.sb(es, "cv", [128, 16])
            junk16 = self.sb(es, "junk16", [128, 16])
            negm = self.sb(es, "negm", [128, 8])
            Zs = self.sb(es, "Zs", [128, 8])
            gA = [self.sb(es, "gA", [128, EW], BF16) for _ in range(2)]
            candb = [self.sb(es, "candb", [128, EW]) for _ in range(4)]
            EE = [self.sb(es, "EE", [128, EW], BF16) for _ in range(4)]
            Ghs = [[self.sb(es, "Ghs", [128, EW], BF16) for _ in range(8)] for _ in range(2)]
            WT = [self.sb(es, "WT", [128, 4, 128], BF16) for _ in range(2)]
            ntmp = {"xs": self.sb(es, "xsE", [128, D]), "junk": self.sb(es, "junkE", [128, D], BF16), "ss": self.sb(es, "ssE", [128, 4])}
            ssf = self.sb(es, "ssf", [128, 1])
            S.dma(lambda e: e.dma_start(out=keysT[:], in_=io["keysT"]), writes=["keysT"])
            uT_bf, ukeys = self.wsrc(io["uT"])
            pv_bf, vkeys = self.wsrc(io["pv"])
            uview = uT_bf.rearrange("(kc p) c -> p kc c", p=128)

            def load_U(eb):
                sl = eb % 2
                S.dma(lambda e, eb=eb, sl=sl: e.dma_start(out=slU[sl][:], in_=uview[:, :, eb * EW:(eb + 1) * EW]),
                      reads=ukeys, writes=[("slU", sl)], q="sp")

            def load_V(eb):
                sl = eb % 2
                S.dma(lambda e, eb=eb, sl=sl: e.dma_start(out=slV[sl][:], in_=pv_bf[eb * EW:(eb + 1) * EW, :].rearrange("(ec p) c -> p ec c", p=128)),
                      reads=vkeys, writes=[("slV", sl)], q="sp")

            def load_slabs(eb):
                load_U(eb)
                load_V(eb)

            for blk in range(SEQ // n):
                toks = [blk * TB + i for i in range(TB)]
                self.ps_rot = [0, 1, 2, 3]
                for i, t in enumerate(toks):
                    S.dma(lambda e, i=i, t=t: e.dma_start(out=x1[i][:], in_=io["s_x1"][b, t * 128:(t + 1) * 128, :]), writes=["x1e"])
                    self.norm_to_T(es, "x1e", x1[i][:], P["A2"], 3, b,
                                   lambda kc, i=i: hx2T[:, kc, i * 128:(i + 1) * 128], ["hx2T"], ntmp)
                for s4 in range(4):
                    sl = s4 % 2
                    self.load_w(slU[sl], ("slU", sl), io["wq"], s4 * 512, 512)
                    for q in range(4):
                        j = s4 * 4 + q
                        pb = self.ps_next()
                        for kc in range(KC):
                            S.op("pe", lambda e, pb=pb, kc=kc, q=q, sl=sl: e.matmul(self.ps[pb][:, 0:n], slU[sl][:, kc, q * 128:(q + 1) * 128], hx2T[:, kc, :],
                                                                                     start=(kc == 0), stop=(kc == KC - 1)), reads=[("slU", sl), "hx2T"], writes=[("ps", pb)])
                        S.op("act", lambda e, pb=pb, j=j: e.copy(qTp[:, j, :], self.ps[pb][:, 0:n]), reads=[("ps", pb)], writes=["qTp"])
                load_slabs(0)
                for i in range(TB):
                    for j4 in range(4):
                        pb = self.ps_next()
                        for q in range(4):
                            j = j4 * 4 + q
                            S.op("pe", lambda e, pb=pb, q=q, j=j, i=i: e.matmul(self.ps[pb][:, q * 128:(q + 1) * 128], qTp[:, j, i * 128:(i + 1) * 128], keysT[:, j, :],
                                                                                 start=True, stop=True), reads=["qTp", "keysT"], writes=[("ps", pb)])
                        S.op("act", lambda e, pb=pb, j4=j4, i=i: e.copy(sc[i][:, j4 * 4:(j4 + 1) * 4, :], self.ps[pb][:, :].rearrange("p (q k) -> p q k", q=4)),
                             reads=[("ps", pb)], writes=[("sc", i)])
                    for j in range(16):
                        S.op("dve", lambda e, j=j, i=i: e.max(out=sv[:, j, 0:8], in_=sc[i][:, j, :]), reads=[("sc", i)], writes=["sv"])
                        S.op("dve", lambda e, j=j, i=i: e.match_replace(out=tmp128[:], in_to_replace=sv[:, j, 0:8], in_values=sc[i][:, j, :], imm_value=NEG),
                             reads=[("sc", i), "sv"], writes=["tmp128"])
                        S.op("dve", lambda e, j=j: e.max(out=sv[:, j, 8:16], in_=tmp128[:]), reads=["tmp128"], writes=["sv"])
                    for h in range(8):
                        S.op("pool", lambda e, h=h: e.tensor_tensor(
                            out=cand[:].rearrange("p (a c) -> p a c", a=16),
                            in0=sv[:, 2 * h, :].unsqueeze(2).to_broadcast([128, 16, 16]),
                            in1=sv[:, 2 * h + 1, :].unsqueeze(1).to_broadcast([128, 16, 16]), op=ALU.add), reads=["sv"], writes=["cand"])
                        S.op("dve", lambda e: e.max(out=cv[:, 0:8], in_=cand[:]), reads=["cand"], writes=["cv"])
                        S.op("dve", lambda e: e.match_replace(out=cand2[:], in_to_replace=cv[:, 0:8], in_values=cand[:], imm_value=NEG),
                             reads=["cand", "cv"], writes=["cand2"])
                        S.op("dve", lambda e: e.max(out=cv[:, 8:16], in_=cand2[:]), reads=["cand2"], writes=["cv"])
                        S.op("dve", lambda e, h=h, i=i: e.tensor_copy(thr[i][:, h:h + 1], cv[:, 15:16]), reads=["cv"], writes=[("thr", i)])
                        S.op("dve", lambda e, h=h: e.tensor_scalar(out=negm[:, h:h + 1], in0=cv[:, 0:1], scalar1=-1.0, scalar2=None, op0=ALU.mult),
                             reads=["cv"], writes=["negm"])
                        S.op("act", lambda e, h=h: e.activation(out=junk16[:], in_=cv[:], func=AF.Exp, bias=negm[:, h:h + 1], accum_out=Zs[:, h:h + 1]),
                             reads=["cv", "negm"], writes=["junk16", "Zs"])
                    S.op("act", lambda e: e.activation(out=Zs[:], in_=Zs[:], func=AF.Ln), reads=["Zs"], writes=["Zs"])
                    S.op("dve", lambda e, i=i: e.tensor_tensor(out=c0t[i][:], in0=negm[:], in1=Zs[:], op=ALU.subtract), reads=["negm", "Zs"], writes=[("c0t", i)])
                pairs = [(eb, i) for eb in range(NEB) for i in range(TB)]
                ptb = [self.ps[2][:, :].bitcast(BF16), self.ps[3][:, :].bitcast(BF16)]

                def emit_A(pi):
                    eb, i = pairs[pi]
                    pa = pi % 2
                    sl = eb % 2
                    for ec in range(4):
                        for kc in range(KC):
                            S.op("pe", lambda e, pa=pa, kc=kc, i=i, sl=sl, ec=ec: e.matmul(
                                self.ps[pa][:, ec * 128:(ec + 1) * 128], slU[sl][:, kc, ec * 128:(ec + 1) * 128], hx2T[:, kc, i * 128:(i + 1) * 128],
                                start=(kc == 0), stop=(kc == KC - 1)), reads=["hx2T", ("slU", sl)], writes=[("ps", pa)])

                def emit_gelu(pi):
                    pa = pi % 2
                    S.op("act", lambda e, pa=pa: e.activation(out=gA[pa][:], in_=self.ps[pa][:, :], func=AF.Gelu), reads=[("ps", pa)], writes=[("gA", pa)])

                def tail_T(pj):
                    pa = pj % 2
                    for ec in range(4):
                        for h in range(8):
                            S.op("pe", lambda e, ec=ec, pa=pa, h=h: e.matmul(
                                self.ps[2 + pa][:, ec * 128:(ec + 1) * 128], Ghs[pa][h][:, ec * 128:(ec + 1) * 128], self.cstb(0),
                                start=(h == 0), stop=(h == 7)), reads=[("Ghs", pa, h), "cbf"], writes=[("ps", 2 + pa)])
                    S.op("dve", lambda e, pa=pa: e.tensor_tensor(out=WT[pa][:].rearrange("p c t -> p (c t)"), in0=self.ps[2 + pa][:, :], in1=gA[pa][:], op=ALU.mult),
                         reads=[("ps", 2 + pa), ("gA", pa)], writes=[("WT", pa)])

                def tail_y(pj):
                    eb, i = pairs[pj]
                    pa = pj % 2
                    sl = eb % 2
                    for cb4 in range(4):
                        for ec in range(4):
                            S.op("pe", lambda e, cb4=cb4, ec=ec, pa=pa, sl=sl: e.matmul(self.ps[4 + cb4][:, :], WT[pa][:, ec, :], slV[sl][:, ec, cb4 * 512:(cb4 + 1) * 512],
                                                                                       start=(ec == 0), stop=(ec == 3)), reads=[("WT", pa), ("slV", sl)], writes=[("ps", 4 + cb4)])

                def tail_acc(pj):
                    eb, i = pairs[pj]
                    for cb4 in range(4):
                        if eb == 0:
                            S.op("act", lambda e, cb4=cb4, i=i: e.copy(yacc[i][:, cb4 * 512:(cb4 + 1) * 512], self.ps[4 + cb4][:, :]),
                                 reads=[("ps", 4 + cb4)], writes=[("yacc", i, cb4)])
                        else:
                            S.op("dve", lambda e, cb4=cb4, i=i: e.tensor_tensor(out=yacc[i][:, cb4 * 512:(cb4 + 1) * 512], in0=yacc[i][:, cb4 * 512:(cb4 + 1) * 512],
                                                                                   in1=self.ps[4 + cb4][:, :], op=ALU.add), reads=[("ps", 4 + cb4), ("yacc", i, cb4)], writes=[("yacc", i, cb4)])

                emit_A(0)
                emit_gelu(0)
                for pi, (eb, i) in enumerate(pairs):
                    pa = pi % 2
                    sl = eb % 2
                    if i == 0 and eb + 1 < NEB:
                        load_U(eb + 1)
                    for h in range(8):
                        hb = h % 4
                        ceng = "pool"
                        S.op(ceng, lambda e, h=h, i=i, eb=eb, hb=hb: e.tensor_tensor(
                            out=candb[hb][:].rearrange("p (a k) -> p a k", a=4),
                            in0=sc[i][:, 2 * h, eb * 4:(eb + 1) * 4].unsqueeze(2).to_broadcast([128, 4, 128]),
                            in1=sc[i][:, 2 * h + 1, :].unsqueeze(1).to_broadcast([128, 4, 128]), op=ALU.add), reads=[("sc", i)], writes=[("candb", hb)])
                        S.op("act", lambda e, h=h, i=i, hb=hb: e.activation(out=EE[hb][:], in_=candb[hb][:], func=AF.Exp, bias=c0t[i][:, h:h + 1]),
                             reads=[("candb", hb), ("c0t", i)], writes=[("EE", hb)])
                        S.op("dve", lambda e, h=h, i=i, hb=hb, pa=pa: e.scalar_tensor_tensor(out=Ghs[pa][h][:], in0=candb[hb][:], scalar=thr[i][:, h:h + 1], in1=EE[hb][:],
                                                                                             op0=ALU.is_ge, op1=ALU.mult), reads=[("candb", hb), ("EE", hb), ("thr", i)], writes=[("Ghs", pa, h)])
                        if h == 1:
                            if pi > 0:
                                tail_T(pi - 1)
                            if pi + 1 < len(pairs):
                                emit_A(pi + 1)
                        if h == 3:
                            if pi > 0:
                                tail_y(pi - 1)
                            if i == 0 and eb + 1 < NEB:
                                load_V(eb + 1)
                        if pi > 0 and h == 6:
                            tail_acc(pi - 1)
                    if pi + 1 < len(pairs):
                        emit_gelu(pi + 1)
                last = len(pairs) - 1
                tail_T(last)
                tail_y(last)
                tail_acc(last)
                self.ps_rot = list(range(8))
                for i, t in enumerate(toks):
                    S.dma(lambda e, i=i, t=t: e.dma_start(out=x1[i][:], in_=io["s_x1"][b, t * 128:(t + 1) * 128, :]), writes=["x1e"])
                    S.op("dve", lambda e, i=i: e.tensor_tensor(out=yacc[i][:], in0=yacc[i][:], in1=g2bc[:], op=ALU.mult), reads=[("yacc", i, 0), ("yacc", i, 1), ("yacc", i, 2), ("yacc", i, 3), "g2bc"], writes=[("yacc", i, 0), ("yacc", i, 1), ("yacc", i, 2), ("yacc", i, 3)])
                    S.op("dve", lambda e, i=i: e.tensor_tensor(out=x1[i][:], in0=x1[i][:], in1=yacc[i][:], op=ALU.add), reads=[("yacc", i, 0), ("yacc", i, 1), ("yacc", i, 2), ("yacc", i, 3), "x1e"], writes=["x1e"])
                    S.op("act", lambda e, i=i: e.activation(out=yacc[i][:], in_=x1[i][:], func=AF.Square, accum_out=ssf[:, 0:1]), reads=["x1e"], writes=[("yacc", i, 0), ("yacc", i, 1), ("yacc", i, 2), ("yacc", i, 3), "ssf"])
                    self.rstd_from_ss(ssf, D, "ssf", 1)
                    S.op("dve", lambda e, i=i: e.scalar_tensor_tensor(out=yacc[i][:], in0=x1[i][:], scalar=ssf[:, 0:1], in1=fnw_bc[:], op0=ALU.mult, op1=ALU.mult),
                         reads=["x1e", "ssf", "fnw_bc"], writes=[("yacc", i, 0), ("yacc", i, 1), ("yacc", i, 2), ("yacc", i, 3)])
                    S.dma(lambda e, i=i, t=t: e.dma_start(out=io["out"][b, t * 128:(t + 1) * 128, :], in_=yacc[i][:]), reads=[("yacc", i, 0), ("yacc", i, 1), ("yacc", i, 2), ("yacc", i, 3)], writes=[("out", b, t)])
        S.barrier()


def _consts():
    t = np.arange(128)[:, None]
    i = np.arange(128)[None, :]
    c = np.zeros((128, 8, 128), np.float32)
    c[:, 0] = (t == i)
    c[:, 1] = (t <= i)
    c[:, 2] = (t >= i)
    c[:, 3] = (t > i)
    c[:, 4] = (t < i)
    c[:, 5] = (t <= i)
    c[:, 6] = (t >= i)
    c[:, 7] = 1.0
    return c


def _col(v, n):
    return np.ascontiguousarray(np.asarray(v, np.float32).reshape(n, 128).T)


def make_in_maps(inputs, n_cores=8, nb=2):
    g = {k: np.asarray(v) for k, v in inputs.items()}
    shared = {
        "w_ada": np.ascontiguousarray(g["w_ada"][0]),
        "b_adaT": _col(g["b_ada"][0], 96),
        "n1T": _col(g["norm1_w"][0], KC),
        "n2T": _col(g["norm2_w"][0], KC),
        "w_in": np.ascontiguousarray(g["w_in"][0]),
        "w2f": np.ascontiguousarray(np.concatenate([g["w_lr2_f"][0], g["b_lr_f"][0][None]], 0)),
        "w2b": np.ascontiguousarray(np.concatenate([g["w_lr2_b"][0], g["b_lr_b"][0][None]], 0)),
        "gnwT": _col(g["gla_norm_w"][0], 4),
        "snwT": _col(g["ssd_norm_w"][0], 32),
        "fnw": np.ascontiguousarray(g["final_norm_w"][None]),
        "conv_wT": np.ascontiguousarray(g["conv_w"][0].reshape(9, 48, 128).transpose(2, 1, 0)),
        "conv_bT": _col(g["conv_b"][0], 48),
        "ssd_rows": np.ascontiguousarray(np.concatenate(
            [g["a_log_f"][0], g["a_log_b"][0], g["d_skip"][0], g["dt_bias_f"][0], g["dt_bias_b"][0]])[None]),
        "b_gateT": _col(g["b_gate"][0], 32),
        "w_gla_out": np.ascontiguousarray(g["w_gla_out"][0]),
        "w_ssd_out": np.ascontiguousarray(g["w_ssd_out"][0]),
        "w_o": np.ascontiguousarray(g["w_o"][0]),
        "wq": np.ascontiguousarray(g["peer_wq"][0]),
        "keysT": np.ascontiguousarray(g["peer_keys"][0].reshape(16, 128, 128).transpose(2, 0, 1)),
        "uT": np.ascontiguousarray(g["peer_u"][0].T),
        "pv": np.ascontiguousarray(g["peer_v"][0]),
        "consts": _consts(),
    }
    maps = []
    for i in range(n_cores):
        bs = slice(i * nb, (i + 1) * nb)
        cvecs = np.stack([g["c"][i * nb + k] for k in range(nb)] + [g["c_ctx"]] * (3 - nb), -1)
        m = dict(shared)
        m["x"] = np.ascontiguousarray(g["x"][bs])
        m["ctx"] = np.ascontiguousarray(g["ctx"][bs])
        m["cT"] = np.ascontiguousarray(cvecs.reshape(KC, 128, 3).transpose(1, 0, 2).astype(np.float32))
        maps.append(m)
    return maps


def kernel(**inputs):
    n = 8
    nc = K(nb=2).build()
    maps = make_in_maps(inputs, n, 2)
    res = run_bass_kernel_spmd(nc, maps, core_ids=list(range(n)))
    return np.concatenate([r["out"] for r in res.results], axis=0).astype(np.float32)
```

```python
from contextlib import ExitStack
import numpy as np
import concourse.bass as bass
import concourse.mybir as mybir
from concourse.bass_utils import run_bass_kernel_spmd

F32 = mybir.dt.float32
BF16 = mybir.dt.bfloat16
AF = mybir.ActivationFunctionType
ALU = mybir.AluOpType

N_DMA_SEMS = 24
D = 2048
KC = 16
SEQ = 2048
CTXL = 256
LTOT = SEQ + CTXL
NT = LTOT // 128
NTC = CTXL // 128
EPS = 1e-6
C_Q, C_K, C_V, C_R, C_LRF, C_LRB, C_Z = 0, 1024, 2048, 4096, 6144, 6160, 6176
C_XS, C_B, C_C, C_DT, C_GL = 10272, 14368, 15392, 16416, 16544
IN_COLS = 20640


class Sched:
    ENGS = ("pe", "act", "dve", "pool", "sp")

    def __init__(self, nc, es):
        self.nc = nc
        self.q = {e: [] for e in self.ENGS}
        self.sem = {e: es.enter_context(nc.semaphore("s_" + e)) for e in self.ENGS}
        self.cnt = {e: 0 for e in self.ENGS}
        self.seen = {e: {} for e in self.ENGS}
        self.dsem = [es.enter_context(nc.semaphore("d_%d" % i)) for i in range(N_DMA_SEMS)]
        self.dcnt = [0] * N_DMA_SEMS
        self.dnext = 0
        self.last_w = {}
        self.readers = {}
        self.n_inst = 0
        self.n_wait = 0
        self.clock = {}
        self.gidx = 0

    def _deps(self, reads, writes):
        deps = set()
        for b in reads:
            t = self.last_w.get(b)
            if t is not None:
                deps.add(t)
        for b in writes:
            t = self.last_w.get(b)
            if t is not None:
                deps.add(t)
            for r in self.readers.get(b, ()):
                deps.add(r)
        return deps

    def _emit_waits(self, e, deps, skip_self_pe=True):
        seen = self.seen[e]
        order = sorted(deps, key=lambda t: -self.clock[t][0] if t in self.clock else 0)
        for (k, v) in order:
            if e == "pe" and k == "pe" and skip_self_pe:
                continue
            if seen.get(k, 0) >= v:
                continue
            seen[k] = v
            sem = self.sem[k] if isinstance(k, str) else self.dsem[k]
            self.q[e].append(("wait", sem, v))
            self.n_wait += 1
            c = self.clock.get((k, v))
            if c is not None:
                for kk, vv in c[1].items():
                    if seen.get(kk, 0) < vv:
                        seen[kk] = vv

    def _publish(self, tok, e):
        self.gidx += 1
        self.clock[tok] = (self.gidx, dict(self.seen[e]))

    def _record(self, tok, reads, writes):
        for b in writes:
            self.last_w[b] = tok
            self.readers[b] = []
        for b in reads:
            if b in writes:
                continue
            self.readers.setdefault(b, []).append(tok)

    def op(self, e, fn, reads=(), writes=()):
        deps = self._deps(reads, writes)
        self._emit_waits(e, deps)
        self.cnt[e] += 1
        tok = (e, self.cnt[e])
        self._publish(tok, e)
        self.q[e].append(("op", fn, self.sem[e]))
        self._record(tok, reads, writes)
        self.n_inst += 1
        return tok

    def dma(self, fn, reads=(), writes=(), q="sp"):
        deps = self._deps(reads, writes)
        i = self.dnext
        self.dnext = (self.dnext + 1) % N_DMA_SEMS
        if self.dcnt[i] > 0:
            deps.add((i, self.dcnt[i]))
        self._emit_waits(q, deps, skip_self_pe=False)
        self.dcnt[i] += 16
        tok = (i, self.dcnt[i])
        self._publish(tok, q)
        self.q[q].append(("dma", fn, self.dsem[i]))
        self._record(tok, reads, writes)
        self.n_inst += 1
        return tok

    def wait_all(self, e):
        deps = set()
        for k in self.ENGS:
            if self.cnt[k] > 0:
                deps.add((k, self.cnt[k]))
        for i in range(N_DMA_SEMS):
            if self.dcnt[i] > 0:
                deps.add((i, self.dcnt[i]))
        self._emit_waits(e, deps, skip_self_pe=False)

    def barrier(self):
        for e in self.ENGS:
            self.wait_all(e)
        self.last_w = {}
        self.readers = {}
        self.clock = {}

    def emit(self):
        nc = self.nc
        with nc.Block() as block:
            def run(e):
                def body(eng):
                    for it in self.q[e]:
                        if it[0] == "wait":
                            eng.wait_ge(it[1], it[2])
                        elif it[0] == "op":
                            it[1](eng).then_inc(it[2], 1)
                        else:
                            it[1](eng).then_inc(it[2], 16)
                return body
            block.tensor(run("pe"))
            block.scalar(run("act"))
            block.vector(run("dve"))
            block.gpsimd(run("pool"))
            block.sync(run("sp"))


class K:
    def __init__(self, nb=2, stages="0ABCDE", dbg=None):
        self.nb = nb
        self.stages = stages
        self.dbg = dbg or {}
        self.uid = 0

    def sb(self, es, name, shape, dt=F32):
        self.uid += 1
        return es.enter_context(self.nc.sbuf_tensor("%s_%d" % (name, self.uid), list(shape), dt))

    def ps_next(self):
        i = self.ps_rot[self.ps_i % len(self.ps_rot)]
        self.ps_i += 1
        return i

    def build(self):
        nc = bass.Bass("TRN2", target_bir_lowering=False)
        self.nc = nc
        nb = self.nb

        def din(name, shape, dt=F32):
            return nc.dram_tensor(name, list(shape), dt, kind="ExternalInput").ap()

        io = {}
        io["x"] = din("x", [nb, SEQ, D])
        io["ctx"] = din("ctx", [nb, CTXL, D])
        io["cT"] = din("cT", [128, KC, 3])
        io["w_ada"] = din("w_ada", [D, 6 * D])
        io["b_adaT"] = din("b_adaT", [128, 96])
        io["n1T"] = din("n1T", [128, KC])
        io["n2T"] = din("n2T", [128, KC])
        io["w_in"] = din("w_in", [D, IN_COLS])
        io["w2f"] = din("w2f", [17, 1024])
        io["w2b"] = din("w2b", [17, 1024])
        io["gnwT"] = din("gnwT", [128, 4])
        io["snwT"] = din("snwT", [128, 32])
        io["fnw"] = din("fnw", [1, D])
        io["conv_wT"] = din("conv_wT", [128, 48, 9])
        io["conv_bT"] = din("conv_bT", [128, 48])
        io["ssd_rows"] = din("ssd_rows", [1, 5 * 64])
        io["b_gateT"] = din("b_gateT", [128, 32])
        io["w_gla_out"] = din("w_gla_out", [D, D])
        io["w_ssd_out"] = din("w_ssd_out", [2 * D, D])
        io["w_o"] = din("w_o", [D, D])
        io["wq"] = din("wq", [D, D])
        io["keysT"] = din("keysT", [128, 16, 128])
        io["uT"] = din("uT", [D, 16384])
        io["pv"] = din("pv", [16384, D])
        io["consts"] = din("consts", [128, 8, 128])
        io["out"] = nc.dram_tensor("out", [nb, SEQ, D], F32, kind="ExternalOutput").ap()
        skind = "ExternalOutput" if self.dbg.get("scratch_out") else "Internal"
        io["s_og"] = nc.dram_tensor("s_og", [nb, SEQ, D], F32, kind=skind).ap()
        io["s_ys"] = nc.dram_tensor("s_ys", [nb, SEQ, 2 * D], F32, kind=skind).ap()
        io["s_x1"] = nc.dram_tensor("s_x1", [nb, SEQ, D], F32, kind=skind).ap()
        io["s_og2"] = nc.dram_tensor("s_og2", [nb, SEQ, D], F32, kind=skind).ap()
        io["s_ys2"] = nc.dram_tensor("s_ys2", [nb, SEQ, 2 * D], F32, kind=skind).ap()
        for name, shape in self.dbg.get("outs", {}).items():
            io[name] = nc.dram_tensor(name, list(shape), F32, kind="ExternalOutput").ap()
        self.io = io

        with ExitStack() as es:
            S = Sched(nc, es)
            self.S = S
            self.ps = [es.enter_context(nc.psum_tensor("ps%d" % i, [128, 512], F32)) for i in range(8)]
            self.ps_rot = list(range(8))
            self.ps_i = 0
            P = {}
            P["consts"] = self.sb(es, "consts", [128, 8, 128])
            P["cbf"] = self.sb(es, "cbf", [128, 8, 128], BF16)
            P["modT"] = self.sb(es, "modT", [128, 96, 3])
            P["A1"] = self.sb(es, "A1", [128, KC, 3])
            P["A2"] = self.sb(es, "A2", [128, KC, 3])
            P["ones_bf"] = self.sb(es, "ones_bf", [128, 128], BF16)
            self.P = P
            S.dma(lambda e: e.dma_start(out=P["consts"][:], in_=io["consts"]), writes=["consts"])
            S.op("dve", lambda e: e.tensor_copy(P["cbf"][:], P["consts"][:]), reads=["consts"], writes=["cbf"])
            S.op("dve", lambda e: e.memset(P["ones_bf"][:], 1.0), writes=["ones_bf"])

            self.precast()
            if "0" in self.stages:
                self.stage0()
            for b in range(nb):
                with ExitStack() as es_seq:
                    S.barrier()
                    self.hT = self.sb(es_seq, "hT", [128, KC, LTOT], BF16)
                    if "A" in self.stages:
                        self.stageA(b)
                    if "B" in self.stages:
                        self.stageB(b)
                    if "C" in self.stages:
                        self.stageC(b)
                    if "D" in self.stages:
                        self.stageD(b)
                S.barrier()
                if "E" in self.stages:
                    self.stageE(b)
            S.wait_all("sp")
            S.emit()
        return nc

    def cst(self, i):
        return self.P["consts"][:, i, :]

    def cstb(self, i):
        return self.P["cbf"][:, i, :]

    def stage0(self):
        nc, S, io, P = self.nc, self.S, self.io, self.P
        with ExitStack() as es:
            cT = self.sb(es, "cT", [128, KC, 3])
            sc = self.sb(es, "sc", [128, KC, 3])
            badT = self.sb(es, "badT", [128, 96])
            n1T = self.sb(es, "n1T", [128, KC])
            n2T = self.sb(es, "n2T", [128, KC])
            slab = [self.sb(es, "adaslab", [128, KC, 512]) for _ in range(2)]
            S.dma(lambda e: e.dma_start(out=cT[:], in_=io["cT"]), writes=["cT"])
            S.dma(lambda e: e.dma_start(out=badT[:], in_=io["b_adaT"]), writes=["badT"])
            S.dma(lambda e: e.dma_start(out=n1T[:], in_=io["n1T"]), writes=["n1T"])
            S.dma(lambda e: e.dma_start(out=n2T[:], in_=io["n2T"]), writes=["n2T"])
            S.op("act", lambda e: e.activation(out=sc[:], in_=cT[:], func=AF.Silu), reads=["cT"], writes=["sc"])
            wv = io["w_ada"].rearrange("(kc p) c -> p kc c", p=128)
            pb = self.ps_next()
            psm = self.ps[pb]
            for blk in range(24):
                sl = slab[blk % 2]
                key = "adaslab%d" % (blk % 2)
                S.dma(lambda e, sl=sl, blk=blk: e.dma_start(out=sl[:], in_=wv[:, :, blk * 512:(blk + 1) * 512]),
                      writes=[key])
                for cc in range(4):
                    c = blk * 4 + cc
                    for kc in range(KC):
                        S.op("pe", lambda e, sl=sl, cc=cc, kc=kc, c=c: e.matmul(
                            psm[:, c * 3:c * 3 + 3], sl[:, kc, cc * 128:(cc + 1) * 128], sc[:, kc, :],
                            start=(kc == 0), stop=(kc == KC - 1)),
                            reads=[key, "sc"], writes=[("ps", pb)])
            modT = P["modT"]
            S.op("dve", lambda e: e.tensor_tensor(
                out=modT[:], in0=psm[:, 0:288].rearrange("p (c j) -> p c j", j=3),
                in1=badT[:].unsqueeze(2).to_broadcast([128, 96, 3]), op=ALU.add),
                reads=[("ps", pb), "badT"], writes=["modT"])
            for (A, nT, key, c0, akey) in ((P["A1"], n1T, "n1T", 16, "A1"), (P["A2"], n2T, "n2T", 64, "A2")):
                S.op("dve", lambda e, A=A, nT=nT, c0=c0: e.scalar_tensor_tensor(
                    out=A[:], in0=modT[:, c0:c0 + KC, :], scalar=1.0,
                    in1=nT[:].unsqueeze(2).to_broadcast([128, KC, 3]), op0=ALU.add, op1=ALU.mult),
                    reads=["modT", key], writes=[akey])
            self.S.barrier()

    def mod_col(self, which, kc, j):
        return self.P["modT"][:, which * KC + kc, j:j + 1]

    def norm_to_T(self, es_tmp, src_key, src, A, SH_which, j, dst_fn, dst_keys, tmp):
        S = self.S
        xs, junk, ss = tmp["xs"], tmp["junk"], tmp["ss"]
        S.op("act", lambda e: e.activation(out=junk[:], in_=src, func=AF.Square, accum_out=ss[:, 0:1]),
             reads=[src_key], writes=["nt_junk", "nt_ss"])
        S.op("dve", lambda e: e.tensor_scalar(out=ss[:, 1:2], in0=ss[:, 0:1], scalar1=1.0 / D, scalar2=EPS,
                                               op0=ALU.mult, op1=ALU.add), reads=["nt_ss"], writes=["nt_ss1"])
        S.op("act", lambda e: e.activation(out=ss[:, 2:3], in_=ss[:, 1:2], func=AF.Sqrt), reads=["nt_ss1"], writes=["nt_ss2"])
        S.op("dve", lambda e: e.reciprocal(out=ss[:, 3:4], in_=ss[:, 2:3]), reads=["nt_ss2"], writes=["nt_rstd"])
        S.op("dve", lambda e: e.tensor_scalar(out=xs[:], in0=src, scalar1=ss[:, 3:4], scalar2=None, op0=ALU.mult),
             reads=[src_key, "nt_rstd"], writes=["nt_xs"])
        for g4 in range(4):
            pb = self.ps_next()
            for q in range(4):
                kc = g4 * 4 + q
                S.op("pe", lambda e, pb=pb, q=q, kc=kc: e.transpose(
                    self.ps[pb][:, q * 128:(q + 1) * 128], xs[:, kc * 128:(kc + 1) * 128], self.cst(0)),
                    reads=["nt_xs", "consts"], writes=[("ps", pb)])
            for q in range(4):
                kc = g4 * 4 + q
                S.op("dve", lambda e, pb=pb, q=q, kc=kc: e.tensor_scalar(
                    out=dst_fn(kc), in0=self.ps[pb][:, q * 128:(q + 1) * 128],
                    scalar1=A[:, kc, j:j + 1], scalar2=self.mod_col(SH_which, kc, j), op0=ALU.mult, op1=ALU.add),
                    reads=[("ps", pb), "modT"], writes=dst_keys)

    def stageA(self, b):
        S, io = self.S, self.io
        with ExitStack() as es:
            xt = [self.sb(es, "xt", [128, D]) for _ in range(2)]
            tmp = {"xs": self.sb(es, "xs", [128, D]), "junk": self.sb(es, "junk", [128, D]),
                   "ss": self.sb(es, "ss", [128, 4])}
            for t in range(NT):
                buf = xt[t % 2]
                key = "xt%d" % (t % 2)
                if t < NTC:
                    src = io["ctx"][b, t * 128:(t + 1) * 128, :]
                    j = 2
                else:
                    src = io["x"][b, (t - NTC) * 128:(t - NTC + 1) * 128, :]
                    j = b
                S.dma(lambda e, buf=buf, src=src: e.dma_start(out=buf[:], in_=src), writes=[key])
                self.norm_to_T(es, key, buf[:], self.P["A1"], 0, j,
                               lambda kc, t=t: self.hT[:, kc, t * 128:(t + 1) * 128], [("hT", t)], tmp)
            if "hT" in self.dbg.get("outs", {}) and b == 0:
                self.dump_bf16(es, self.hT[:, :, :], [128, KC, LTOT], "hT", [("hT", t) for t in range(NT)])
        S.barrier()

    def dump_bf16(self, es, ap, shape, name, keys):
        S = self.S
        t = self.sb(es, "dump", [128, shape[2]])
        for i in range(shape[1]):
            S.op("dve", lambda e, i=i: e.tensor_copy(t[:], ap[:, i, :]), reads=keys, writes=["dump"])
            S.dma(lambda e, i=i: e.dma_start(out=self.io[name][:, i, :], in_=t[:]), reads=["dump"], writes=["dbg_" + name])

    BIGW = ("w_in", "w_gla_out", "w_ssd_out", "w_o", "wq", "uT", "pv")

    def precast(self):
        nc, S, io = self.nc, self.S, self.io
        self.bf = {}
        self.bfkeys = {}
        for name in self.BIGW:
            src = io[name]
            rows, cols = src.shape
            dst = nc.dram_tensor(name + "_bf", [rows, cols], BF16, kind="Internal").ap()
            self.bf[id(src)] = (name, dst)
            step = 256 if cols > 8192 else 1024
            keys = []
            for r0 in range(0, rows, step):
                k = ("wbf", name, r0)
                keys.append(k)
                self.pc_i = getattr(self, "pc_i", 0) + 1
                S.dma(lambda e, dst=dst, src=src, r0=r0, step=step: e.dma_start(out=dst[r0:r0 + step, :], in_=src[r0:r0 + step, :]),
                      writes=[k, ("pcslot", self.pc_i % 3)], q="pool")
            self.bfkeys[name] = keys

    def wsrc(self, wap):
        name, dst = self.bf[id(wap)]
        return dst, self.bfkeys[name]

    def load_w(self, buf, key, wap, c0, ncols, nk=KC, r0=0, b0=0):
        dst, keys = self.wsrc(wap)
        wv = dst[r0:r0 + nk * 128, :].rearrange("(kc p) c -> p kc c", p=128)
        self.S.dma(lambda e: e.dma_start(out=buf[:, 0:nk, b0:b0 + ncols], in_=wv[:, :, c0:c0 + ncols]),
                   reads=keys, writes=[key], q="sp")

    def tok_blocks(self):
        return [(0, 512), (512, 512), (1024, 512), (1536, 512), (2048, 256)]

    def scan_order(self, d):
        if d == 0:
            return list(range(NT))
        return [1, 0] + list(range(NT - 1, NTC - 1, -1))

    def stageB(self, b):
        S, io, P, hT = self.S, self.io, self.P, self.hT
        with ExitStack() as es:
            wsl = [self.sb(es, "wsl", [128, KC, 512], BF16) for _ in range(2)]
            lrT = [self.sb(es, "lrT", [17, LTOT], BF16) for _ in range(2)]
            w2 = [self.sb(es, "w2", [17, 1024], BF16) for _ in range(2)]
            qT = self.sb(es, "qT", [128, 2, LTOT], BF16)
            kT = self.sb(es, "kT", [128, 2, LTOT], BF16)
            vv = self.sb(es, "vv", [128, NT, 512], BF16)
            ktm = self.sb(es, "ktm", [128, NT, 256], BF16)
            Sst_ = [self.sb(es, "Sst", [128, 2, 512]) for _ in range(2)]
            Sbf_ = [self.sb(es, "Sbf", [128, 2, 512], BF16) for _ in range(2)]
            e1_ = [self.sb(es, "e1", [128, 256]) for _ in range(2)]
            gneg_ = [self.sb(es, "gneg", [128, 256]) for _ in range(2)]
            eq_ = [self.sb(es, "eq", [128, 2, 128]) for _ in range(2)]
            ek_ = [self.sb(es, "ek", [128, 2, 128]) for _ in range(2)]
            ekt_ = [self.sb(es, "ekt", [128, 256]) for _ in range(2)]
            qd_ = [self.sb(es, "qd", [128, 2, 128], BF16) for _ in range(2)]
            kd_ = [self.sb(es, "kd", [128, 2, 128], BF16) for _ in range(2)]
            kt_ = [self.sb(es, "kt", [128, 256], BF16) for _ in range(2)]
            aTm_ = [self.sb(es, "aTm", [128, 128], BF16) for _ in range(2)]
            osb_ = [self.sb(es, "osb", [128, 512]) for _ in range(2)]
            for d, nm in ((0, "w2f"), (1, "w2b")):
                S.dma(lambda e, d=d, nm=nm: e.dma_start(out=w2[d][:], in_=io[nm]), writes=[("w2", d)], q="pool")
                S.op("dve", lambda e, d=d: e.memset(lrT[d][:], 1.0), writes=[("lrT", d)])
            self.load_w(wsl[0], ("wsl", 0), io["w_in"], C_LRF, 32)
            for d in range(2):
                for (t0, n) in self.tok_blocks():
                    pb = self.ps_next()
                    for kc in range(KC):
                        S.op("pe", lambda e, pb=pb, kc=kc, t0=t0, n=n, d=d: e.matmul(
                            self.ps[pb][0:16, 0:n], wsl[0][:, kc, d * 16:(d + 1) * 16], hT[:, kc, t0:t0 + n],
                            start=(kc == 0), stop=(kc == KC - 1)),
                            reads=[("wsl", 0)] + [("hT", t) for t in range(NT)], writes=[("ps", pb)])
                    S.op("act", lambda e, pb=pb, t0=t0, n=n, d=d: e.copy(lrT[d][0:16, t0:t0 + n], self.ps[pb][0:16, 0:n]),
                         reads=[("ps", pb)], writes=[("lrT", d)])
            hkeys = [("hT", t) for t in range(NT)]
            for h in self.dbg.get('gla_heads', range(4)):
                self.load_w(wsl[0], ("wsl", 0), io["w_in"], C_Q + h * 256, 256)
                self.load_w(wsl[0], ("wsl", 0, "b"), io["w_in"], C_K + h * 256, 256, b0=256)
                self.load_w(wsl[1], ("wsl", 1), io["w_in"], C_V + h * 512, 512)
                for cc in range(4):
                    for (t0, n) in self.tok_blocks():
                        pb = self.ps_next()
                        for kc in range(KC):
                            S.op("pe", lambda e, pb=pb, kc=kc, t0=t0, n=n, cc=cc: e.matmul(
                                self.ps[pb][:, 0:n], wsl[0][:, kc, cc * 128:(cc + 1) * 128], hT[:, kc, t0:t0 + n],
                                start=(kc == 0), stop=(kc == KC - 1)),
                                reads=[("wsl", 0), ("wsl", 0, "b")] + hkeys, writes=[("ps", pb)])
                        if cc < 2:
                            S.op("act", lambda e, pb=pb, t0=t0, n=n, cc=cc: e.activation(
                                out=qT[:, cc, t0:t0 + n], in_=self.ps[pb][:, 0:n], func=AF.Copy, scale=1.0 / 16.0),
                                reads=[("ps", pb)], writes=["qT"])
                        else:
                            S.op("dve", lambda e, pb=pb, t0=t0, n=n, cc=cc: e.tensor_copy(
                                kT[:, cc - 2, t0:t0 + n], self.ps[pb][:, 0:n]),
                                reads=[("ps", pb)], writes=["kT"])
                for t in range(NT):
                    pb = self.ps_next()
                    for kc in range(KC):
                        S.op("pe", lambda e, pb=pb, kc=kc, t=t: e.matmul(
                            self.ps[pb][:, :], hT[:, kc, t * 128:(t + 1) * 128], wsl[1][:, kc, :],
                            start=(kc == 0), stop=(kc == KC - 1)),
                            reads=[("wsl", 1), ("hT", t)], writes=[("ps", pb)])
                    S.op("act", lambda e, pb=pb, t=t: e.copy(vv[:, t, :], self.ps[pb][:, :]),
                         reads=[("ps", pb)], writes=[("vv", t)])
                    pb = self.ps_next()
                    for kc in range(KC):
                        S.op("pe", lambda e, pb=pb, kc=kc, t=t: e.matmul(
                            self.ps[pb][:, 0:256], hT[:, kc, t * 128:(t + 1) * 128], wsl[0][:, kc, 256:512],
                            start=(kc == 0), stop=(kc == KC - 1)),
                            reads=[("wsl", 0), ("wsl", 0, "b"), ("hT", t)], writes=[("ps", pb)])
                    S.op("dve", lambda e, pb=pb, t=t: e.tensor_copy(ktm[:, t, :], self.ps[pb][:, 0:256]),
                         reads=[("ps", pb)], writes=[("ktm", t)])
                for d in range(2):
                    S.op("dve", lambda e, d=d: e.memset(Sst_[d][:], 0.0), writes=[("Sst", d)])
                    S.op("dve", lambda e, d=d: e.memset(Sbf_[d][:], 0.0), writes=[("Sbf", d)])
                for step in range(NT):
                    for d in range(2):
                        t = self.scan_order(d)[step]
                        TI = self.cst(1 + d)
                        TS = self.cst(3 + d)
                        MK = self.cst(5 + d)
                        last = 127 if d == 0 else 0
                        Sst, Sbf, e1, gneg, eq, ek, ekt, osb = Sst_[d], Sbf_[d], e1_[d], gneg_[d], eq_[d], ek_[d], ekt_[d], osb_[d]
                        qd, kd, kt, aTm = qd_[d], kd_[d], kt_[d], aTm_[d]
                        isx = t >= NTC
                        tsl = slice(t * 128, (t + 1) * 128)
                        p_lg = self.ps_next()
                        S.op("pe", lambda e, Sst=Sst, Sbf=Sbf, e1=e1, gneg=gneg, eq=eq, ek=ek, ekt=ekt, osb=osb, qd=qd, kd=kd, kt=kt, aTm=aTm, p_lg=p_lg, tsl=tsl, d=d, h=h: e.matmul(
                            self.ps[p_lg][:, 0:256], lrT[d][0:17, tsl], w2[d][0:17, h * 256:(h + 1) * 256],
                            start=True, stop=True), reads=[("lrT", d), ("w2", d)], writes=[("ps", p_lg)])
                        S.op("act", lambda e, Sst=Sst, Sbf=Sbf, e1=e1, gneg=gneg, eq=eq, ek=ek, ekt=ekt, osb=osb, qd=qd, kd=kd, kt=kt, aTm=aTm, p_lg=p_lg: e.activation(out=e1[:], in_=self.ps[p_lg][:, 0:256], func=AF.Exp, scale=-1.0),
                             reads=[("ps", p_lg)], writes=[("e1", d)])
                        S.op("act", lambda e, Sst=Sst, Sbf=Sbf, e1=e1, gneg=gneg, eq=eq, ek=ek, ekt=ekt, osb=osb, qd=qd, kd=kd, kt=kt, aTm=aTm: e.activation(out=gneg[:], in_=e1[:], func=AF.Ln, bias=1.0),
                             reads=[("e1", d)], writes=[("gneg", d)])
                        p_bt = self.ps_next()
                        for cc in range(2):
                            S.op("pe", lambda e, Sst=Sst, Sbf=Sbf, e1=e1, gneg=gneg, eq=eq, ek=ek, ekt=ekt, osb=osb, qd=qd, kd=kd, kt=kt, aTm=aTm, p_bt=p_bt, cc=cc, TI=TI: e.matmul(
                                self.ps[p_bt][:, cc * 128:(cc + 1) * 128], gneg[:, cc * 128:(cc + 1) * 128], TI,
                                start=True, stop=True), reads=[("gneg", d), "consts"], writes=[("ps", p_bt)])
                        p_r = self.ps_next()
                        S.op("pe", lambda e, Sst=Sst, Sbf=Sbf, e1=e1, gneg=gneg, eq=eq, ek=ek, ekt=ekt, osb=osb, qd=qd, kd=kd, kt=kt, aTm=aTm, p_r=p_r, TS=TS: e.matmul(
                            self.ps[p_r][:, 0:256], TS, gneg[:], start=True, stop=True),
                            reads=[("gneg", d), "consts"], writes=[("ps", p_r)])
                        btv = self.ps[p_bt][:, 0:256].rearrange("p (c i) -> p c i", c=2)
                        S.op("act", lambda e, Sst=Sst, Sbf=Sbf, e1=e1, gneg=gneg, eq=eq, ek=ek, ekt=ekt, osb=osb, qd=qd, kd=kd, kt=kt, aTm=aTm, btv=btv: e.activation(out=eq[:], in_=btv, func=AF.Exp, scale=-1.0 / 16.0),
                             reads=[("ps", p_bt)], writes=[("eq", d)])
                        S.op("act", lambda e, Sst=Sst, Sbf=Sbf, e1=e1, gneg=gneg, eq=eq, ek=ek, ekt=ekt, osb=osb, qd=qd, kd=kd, kt=kt, aTm=aTm, btv=btv: e.activation(out=ek[:], in_=btv, func=AF.Exp, scale=1.0 / 16.0),
                             reads=[("ps", p_bt)], writes=[("ek", d)])
                        S.op("act", lambda e, Sst=Sst, Sbf=Sbf, e1=e1, gneg=gneg, eq=eq, ek=ek, ekt=ekt, osb=osb, qd=qd, kd=kd, kt=kt, aTm=aTm, p_r=p_r: e.activation(out=ekt[:], in_=self.ps[p_r][:, 0:256], func=AF.Exp, scale=-1.0 / 16.0),
                             reads=[("ps", p_r)], writes=[("ekt", d)])
                        S.op("dve", lambda e, Sst=Sst, Sbf=Sbf, e1=e1, gneg=gneg, eq=eq, ek=ek, ekt=ekt, osb=osb, qd=qd, kd=kd, kt=kt, aTm=aTm, tsl=tsl: e.tensor_tensor(out=kt[:], in0=ktm[:, tsl.start // 128, :], in1=ekt[:], op=ALU.mult),
                             reads=[("ktm", t), ("ekt", d)], writes=[("kt", d)])
                        if isx:
                            S.op("dve", lambda e, Sst=Sst, Sbf=Sbf, e1=e1, gneg=gneg, eq=eq, ek=ek, ekt=ekt, osb=osb, qd=qd, kd=kd, kt=kt, aTm=aTm, tsl=tsl: e.tensor_tensor(out=qd[:], in0=qT[:, :, tsl], in1=eq[:], op=ALU.mult),
                                 reads=["qT", ("eq", d)], writes=[("qd", d)])
                            S.op("dve", lambda e, Sst=Sst, Sbf=Sbf, e1=e1, gneg=gneg, eq=eq, ek=ek, ekt=ekt, osb=osb, qd=qd, kd=kd, kt=kt, aTm=aTm, tsl=tsl: e.tensor_tensor(out=kd[:], in0=kT[:, :, tsl], in1=ek[:], op=ALU.mult),
                                 reads=["kT", ("ek", d)], writes=[("kd", d)])
                            p_a = self.ps_next()
                            for cc in range(2):
                                S.op("pe", lambda e, Sst=Sst, Sbf=Sbf, e1=e1, gneg=gneg, eq=eq, ek=ek, ekt=ekt, osb=osb, qd=qd, kd=kd, kt=kt, aTm=aTm, p_a=p_a, cc=cc: e.matmul(
                                    self.ps[p_a][:, 0:128], kd[:, cc, :], qd[:, cc, :], start=(cc == 0), stop=(cc == 1)),
                                    reads=[("kd", d), ("qd", d)], writes=[("ps", p_a)])
                            S.op("dve", lambda e, Sst=Sst, Sbf=Sbf, e1=e1, gneg=gneg, eq=eq, ek=ek, ekt=ekt, osb=osb, qd=qd, kd=kd, kt=kt, aTm=aTm, p_a=p_a, MK=MK: e.tensor_tensor(out=aTm[:], in0=self.ps[p_a][:, 0:128], in1=MK, op=ALU.mult),
                                 reads=[("ps", p_a), "consts"], writes=[("aTm", d)])
                            p_o = self.ps_next()
                            S.op("pe", lambda e, Sst=Sst, Sbf=Sbf, e1=e1, gneg=gneg, eq=eq, ek=ek, ekt=ekt, osb=osb, qd=qd, kd=kd, kt=kt, aTm=aTm, p_o=p_o, t=t: e.matmul(self.ps[p_o][:, :], aTm[:], vv[:, t, :], start=True, stop=False),
                                 reads=[("aTm", d), ("vv", t)], writes=[("ps", p_o)])
                            for cc in range(2):
                                S.op("pe", lambda e, Sst=Sst, Sbf=Sbf, e1=e1, gneg=gneg, eq=eq, ek=ek, ekt=ekt, osb=osb, qd=qd, kd=kd, kt=kt, aTm=aTm, p_o=p_o, cc=cc: e.matmul(self.ps[p_o][:, :], qd[:, cc, :], Sbf[:, cc, :], start=False, stop=(cc == 1)),
                                     reads=[("qd", d), ("Sbf", d)], writes=[("ps", p_o)])
                            dst = io["s_og" if d == 0 else "s_og2"][b, (t - NTC) * 128:(t - NTC + 1) * 128, h * 512:(h + 1) * 512]
                            okey = ("s_og", d, t, h)
                            S.op("act", lambda e, Sst=Sst, Sbf=Sbf, e1=e1, gneg=gneg, eq=eq, ek=ek, ekt=ekt, osb=osb, qd=qd, kd=kd, kt=kt, aTm=aTm, p_o=p_o: e.copy(osb[:], self.ps[p_o][:, :]), reads=[("ps", p_o)], writes=[("osb", d)])
                            S.dma(lambda e, Sst=Sst, Sbf=Sbf, e1=e1, gneg=gneg, eq=eq, ek=ek, ekt=ekt, osb=osb, qd=qd, kd=kd, kt=kt, aTm=aTm, dst=dst: e.dma_start(out=dst, in_=osb[:]), reads=[("osb", d)], writes=[okey])
                        for cc in range(2):
                            p_s = self.ps_next()
                            S.op("pe", lambda e, Sst=Sst, Sbf=Sbf, e1=e1, gneg=gneg, eq=eq, ek=ek, ekt=ekt, osb=osb, qd=qd, kd=kd, kt=kt, aTm=aTm, p_s=p_s, cc=cc, t=t: e.matmul(
                                self.ps[p_s][:, :], kt[:, cc * 128:(cc + 1) * 128], vv[:, t, :], start=True, stop=True),
                                reads=[("kt", d), ("vv", t)], writes=[("ps", p_s)])
                            S.op("dve", lambda e, Sst=Sst, Sbf=Sbf, e1=e1, gneg=gneg, eq=eq, ek=ek, ekt=ekt, osb=osb, qd=qd, kd=kd, kt=kt, aTm=aTm, p_s=p_s, cc=cc, last=last: e.scalar_tensor_tensor(
                                out=Sst[:, cc, :], in0=Sst[:, cc, :], scalar=eq[:, cc, last:last + 1], in1=self.ps[p_s][:, :],
                                op0=ALU.mult, op1=ALU.add), reads=[("Sst", d), ("eq", d), ("ps", p_s)], writes=[("Sst", d)])
                        S.op("act", lambda e, Sst=Sst, Sbf=Sbf, e1=e1, gneg=gneg, eq=eq, ek=ek, ekt=ekt, osb=osb, qd=qd, kd=kd, kt=kt, aTm=aTm: e.copy(Sbf[:], Sst[:]), reads=[("Sst", d)], writes=[("Sbf", d)])
        S.barrier()

    def stageC(self, b):
        S, io, P, hT = self.S, self.io, self.P, self.hT
        hkeys = [("hT", t) for t in range(NT)]
        with ExitStack() as es:
            wsl = [self.sb(es, "wslc", [128, KC, 512], BF16) for _ in range(2)]
            rows = self.sb(es, "rows", [1, 320])
            rows_bf = self.sb(es, "rows_bf", [1, 128], BF16)
            negA = self.sb(es, "negA", [128, 128])
            dsk = self.sb(es, "dsk", [128, 64])
            dt_all = self.sb(es, "dt_all", [128, NT, 128])
            dta_all = self.sb(es, "dta_all", [128, NT, 128])
            cw = self.sb(es, "cw", [128, 48, 9])
            cb = self.sb(es, "cb", [128, 48])
            raw = self.sb(es, "raw", [128, LTOT])
            acc = self.sb(es, "acc", [128, LTOT])
            post = raw
            xs_tm = self.sb(es, "xs_tm", [128, NT, 512], BF16)
            Btm = self.sb(es, "Btm", [128, NT, 128], BF16)
            BT = self.sb(es, "BT", [128, LTOT], BF16)
            CT = self.sb(es, "CT", [128, LTOT], BF16)
            Sst = self.sb(es, "SstC", [128, 512])
            Sbf = self.sb(es, "SbfC", [128, 512], BF16)
            X = self.sb(es, "X", [128, 8, 128])
            cum = self.sb(es, "cum", [128, 8])
            seg = self.sb(es, "seg", [128, 8, 128])
            Lm = seg
            cbm = self.sb(es, "cbm", [128, 128])
            MT = self.sb(es, "MT", [128, 8, 128], BF16)
            xdt = self.sb(es, "xdt", [128, 8, 64], BF16)
            wj = self.sb(es, "wj", [128, 8, 64], BF16)
            ecum = self.sb(es, "ecum", [128, 8])
            ecl = self.sb(es, "ecl", [128, 8])
            wdec = self.sb(es, "wdec", [128, 8])
            t1 = self.sb(es, "t1", [128, 512])
            yo = self.sb(es, "yo", [128, 512])
            e1 = self.sb(es, "e1c", [128, 128])

            S.dma(lambda e: e.dma_start(out=rows[:], in_=io["ssd_rows"]), writes=["rows"])
            S.dma(lambda e: e.dma_start(out=cw[:], in_=io["conv_wT"]), writes=["cw"])
            S.dma(lambda e: e.dma_start(out=cb[:], in_=io["conv_bT"]), writes=["cb"])
            S.op("dve", lambda e: e.tensor_copy(rows_bf[:], rows[0:1, 192:320]), reads=["rows"], writes=["rows_bf"])
            pb = self.ps_next()
            S.op("pe", lambda e, pb=pb: e.matmul(self.ps[pb][:, 0:192], P["consts"][0:1, 7, :], rows[0:1, 0:192], start=True, stop=True),
                 reads=["consts", "rows"], writes=[("ps", pb)])
            S.op("act", lambda e, pb=pb: e.activation(out=negA[:], in_=self.ps[pb][:, 0:128], func=AF.Exp), reads=[("ps", pb)], writes=["negA"])
            S.op("dve", lambda e: e.tensor_scalar(out=negA[:], in0=negA[:], scalar1=-1.0, scalar2=None, op0=ALU.mult), reads=["negA"], writes=["negA"])
            S.op("dve", lambda e, pb=pb: e.tensor_copy(dsk[:], self.ps[pb][:, 128:192]), reads=[("ps", pb)], writes=["dsk"])
            self.load_w(wsl[0], ("wslc", 0), io["w_in"], C_DT, 128)
            for t in range(NT):
                pb = self.ps_next()
                for kc in range(KC):
                    S.op("pe", lambda e, pb=pb, kc=kc, t=t: e.matmul(
                        self.ps[pb][:, 0:128], hT[:, kc, t * 128:(t + 1) * 128], wsl[0][:, kc, 0:128],
                        start=(kc == 0), stop=False), reads=[("wslc", 0), ("hT", t)], writes=[("ps", pb)])
                S.op("pe", lambda e, pb=pb: e.matmul(self.ps[pb][:, 0:128], P["ones_bf"][0:1, :], rows_bf[0:1, :], start=False, stop=True),
                     reads=["ones_bf", "rows_bf"], writes=[("ps", pb)])
                S.op("act", lambda e, pb=pb: e.activation(out=e1[:], in_=self.ps[pb][:, 0:128], func=AF.Exp), reads=[("ps", pb)], writes=["e1c"])
                S.op("act", lambda e, t=t: e.activation(out=dt_all[:, t, :], in_=e1[:], func=AF.Ln, bias=1.0), reads=["e1c"], writes=["dt_all"])
                S.op("dve", lambda e, t=t: e.tensor_tensor(out=dta_all[:, t, :], in0=dt_all[:, t, :], in1=negA[:], op=ALU.mult),
                     reads=["dt_all", "negA"], writes=["dta_all"])

            for g in self.dbg.get("ssd_groups", range(8)):
                self.load_w(wsl[0], ("wslc", 0), io["w_in"], C_XS + g * 512, 512)
                self.load_w(wsl[1], ("wslc", 1), io["w_in"], C_B + g * 128, 128)
                self.load_w(wsl[1], ("wslc", 1, "b"), io["w_in"], C_C + g * 128, 128, b0=128)
                for ch in range(6):
                    if ch < 4:
                        wb, wk, c0, gch = wsl[0], ("wslc", 0), ch * 128, (C_XS - C_XS) // 128 + g * 4 + ch
                    elif ch == 4:
                        wb, wk, c0, gch = wsl[1], ("wslc", 1), 0, 32 + g
                    else:
                        wb, wk, c0, gch = wsl[1], ("wslc", 1), 128, 40 + g
                    for (t0, n) in self.tok_blocks():
                        pb = self.ps_next()
                        for kc in range(KC):
                            S.op("pe", lambda e, pb=pb, kc=kc, t0=t0, n=n, wb=wb, c0=c0: e.matmul(
                                self.ps[pb][:, 0:n], wb[:, kc, c0:c0 + 128], hT[:, kc, t0:t0 + n],
                                start=(kc == 0), stop=(kc == KC - 1)), reads=[wk, ("wslc", 1, "b")] + hkeys, writes=[("ps", pb)])
                        S.op("act", lambda e, pb=pb, t0=t0, n=n: e.copy(raw[:, t0:t0 + n], self.ps[pb][:, 0:n]),
                             reads=[("ps", pb)], writes=["raw"])
                    S.op("dve", lambda e, gch=gch: e.tensor_scalar(out=acc[:], in0=raw[:], scalar1=cw[:, gch, 4:5], scalar2=cb[:, gch:gch + 1],
                                                                      op0=ALU.mult, op1=ALU.add), reads=["raw", "cw", "cb"], writes=["acc"])
                    for dx in (-1, 1):
                        o0, o1 = max(0, -dx), CTXL - max(0, dx)
                        S.op("dve", lambda e, gch=gch, dx=dx, o0=o0, o1=o1: e.scalar_tensor_tensor(
                            out=acc[:, o0:o1], in0=raw[:, o0 + dx:o1 + dx], scalar=cw[:, gch, 4 + dx:5 + dx], in1=acc[:, o0:o1],
                            op0=ALU.mult, op1=ALU.add), reads=["raw", "cw", "acc"], writes=["acc"])
                    rawx = raw[:, CTXL:LTOT].rearrange("p (r c) -> p r c", c=64)
                    accx = acc[:, CTXL:LTOT].rearrange("p (r c) -> p r c", c=64)
                    for dy in (-1, 0, 1):
                        for dx in (-1, 0, 1):
                            if dy == 0 and dx == 0:
                                continue
                            r0, r1 = max(0, -dy), 32 - max(0, dy)
                            c0_, c1_ = max(0, -dx), 64 - max(0, dx)
                            tap = (dy + 1) * 3 + (dx + 1)
                            S.op("dve", lambda e, gch=gch, dy=dy, dx=dx, r0=r0, r1=r1, c0_=c0_, c1_=c1_, tap=tap: e.scalar_tensor_tensor(
                                out=accx[:, r0:r1, c0_:c1_], in0=rawx[:, r0 + dy:r1 + dy, c0_ + dx:c1_ + dx],
                                scalar=cw[:, gch, tap:tap + 1], in1=accx[:, r0:r1, c0_:c1_],
                                op0=ALU.mult, op1=ALU.add), reads=["raw", "cw", "acc"], writes=["acc"])
                    if ch < 5:
                        S.op("act", lambda e: e.activation(out=post[:], in_=acc[:], func=AF.Silu), reads=["acc", "raw"], writes=["raw"])
                        if ch == 4:
                            S.op("dve", lambda e: e.tensor_copy(BT[:], post[:]), reads=["raw"], writes=["BT"])
                        for t in range(NT):
                            pb = self.ps_next()
                            S.op("pe", lambda e, pb=pb, t=t: e.transpose(self.ps[pb][:, 0:128], post[:, t * 128:(t + 1) * 128], self.cst(0)),
                                 reads=["raw", "consts"], writes=[("ps", pb)])
                            if ch < 4:
                                S.op("act", lambda e, pb=pb, t=t, ch=ch: e.copy(xs_tm[:, t, ch * 128:(ch + 1) * 128], self.ps[pb][:, 0:128]),
                                     reads=[("ps", pb)], writes=[("xs_tm", t)])
                            else:
                                S.op("act", lambda e, pb=pb, t=t: e.copy(Btm[:, t, :], self.ps[pb][:, 0:128]),
                                     reads=[("ps", pb)], writes=[("Btm", t)])
                    else:
                        S.op("act", lambda e: e.activation(out=CT[:], in_=acc[:], func=AF.Silu), reads=["acc"], writes=["CT"])
                for d in self.dbg.get("ssd_dirs", range(2)):
                    TI = self.cst(1 + d)
                    MK = self.cst(5 + d)
                    last = 127 if d == 0 else 0
                    hc0 = d * 64 + g * 8
                    S.op("dve", lambda e: e.memset(Sst[:], 0.0), writes=["SstC"])
                    S.op("dve", lambda e: e.memset(Sbf[:], 0.0), writes=["SbfC"])
                    for t in self.scan_order(d):
                        isx = t >= NTC
                        tsl = slice(t * 128, (t + 1) * 128)
                        dta = dta_all[:, t, hc0:hc0 + 8]
                        dtv = dt_all[:, t, hc0:hc0 + 8]
                        S.op("pool", lambda e, dta=dta, TI=TI: e.tensor_tensor(
                            out=X[:], in0=dta.unsqueeze(2).to_broadcast([128, 8, 128]), in1=TI.unsqueeze(1).to_broadcast([128, 8, 128]), op=ALU.mult),
                            reads=["dta_all", "consts"], writes=["X"])
                        p_rb = [self.ps_next(), self.ps_next()]
                        for hb in range(2):
                            S.op("pe", lambda e, hb=hb, p_rb=p_rb: e.matmul(
                                self.ps[p_rb[hb]][:, :], self.cst(7), X[:, hb * 4:(hb + 1) * 4, :], start=True, stop=True),
                                reads=["X", "consts"], writes=[("ps", p_rb[hb])])
                        p_cum = self.ps_next()
                        S.op("pe", lambda e, p_cum=p_cum, dta=dta, TI=TI: e.matmul(self.ps[p_cum][:, 0:8], TI, dta, start=True, stop=True),
                             reads=["dta_all", "consts"], writes=[("ps", p_cum)])
                        S.op("dve", lambda e, p_cum=p_cum: e.tensor_copy(cum[:], self.ps[p_cum][:, 0:8]), reads=[("ps", p_cum)], writes=["cum"])
                        rbv = [self.ps[p_rb[hb]][:, :].rearrange("p (h i) -> p h i", h=4) for hb in range(2)]
                        xsv = xs_tm[:, t, :].rearrange("p (h q) -> p h q", h=8)
                        S.op("pool", lambda e, xsv=xsv, dtv=dtv: e.tensor_tensor(out=xdt[:], in0=xsv, in1=dtv.unsqueeze(2).to_broadcast([128, 8, 64]), op=ALU.mult),
                             reads=[("xs_tm", t), "dt_all"], writes=["xdt"])
                        if isx:
                            p_ys = self.ps_next()
                            S.op("pe", lambda e, p_ys=p_ys, tsl=tsl: e.matmul(self.ps[p_ys][:, :], CT[:, tsl], Sbf[:], start=True, stop=True),
                                 reads=["CT", "SbfC"], writes=[("ps", p_ys)])
                        for hb in range(2):
                            S.op("act", lambda e, hb=hb, rbv=rbv, last=last: e.activation(out=ecl[:, hb * 4:(hb + 1) * 4], in_=rbv[hb][:, :, last], func=AF.Exp),
                                 reads=[("ps", p_rb[hb])], writes=["ecl"])
                            S.op("dve", lambda e, hb=hb, rbv=rbv, last=last: e.tensor_tensor(out=wdec[:, hb * 4:(hb + 1) * 4], in0=rbv[hb][:, :, last],
                                                                                 in1=cum[:, hb * 4:(hb + 1) * 4], op=ALU.subtract),
                                 reads=[("ps", p_rb[hb]), "cum"], writes=["wdec"])
                        S.op("act", lambda e: e.activation(out=wdec[:], in_=wdec[:], func=AF.Exp), reads=["wdec"], writes=["wdec"])
                        S.op("pool", lambda e: e.tensor_tensor(out=wj[:], in0=xdt[:], in1=wdec[:].unsqueeze(2).to_broadcast([128, 8, 64]), op=ALU.mult),
                             reads=["xdt", "wdec"], writes=["wj"])
                        p_su = self.ps_next()
                        S.op("pe", lambda e, p_su=p_su, t=t: e.matmul(self.ps[p_su][:, :], Btm[:, t, :], wj[:].rearrange("p h q -> p (h q)"), start=True, stop=True),
                             reads=[("Btm", t), "wj"], writes=[("ps", p_su)])
                        S.op("dve", lambda e: e.tensor_tensor(out=Sst[:].rearrange("p (h q) -> p h q", h=8), in0=Sst[:].rearrange("p (h q) -> p h q", h=8),
                                                              in1=ecl[:].unsqueeze(2).to_broadcast([128, 8, 64]), op=ALU.mult),
                             reads=["SstC", "ecl"], writes=["SstC"])
                        S.op("dve", lambda e, p_su=p_su: e.tensor_tensor(out=Sst[:], in0=Sst[:], in1=self.ps[p_su][:, :], op=ALU.add),
                             reads=["SstC", ("ps", p_su)], writes=["SstC"])
                        S.op("act", lambda e: e.copy(Sbf[:], Sst[:]), reads=["SstC"], writes=["SbfC"])
                        if isx:
                            for hb in range(2):
                                S.op("dve", lambda e, hb=hb, rbv=rbv: e.tensor_tensor(
                                    out=seg[:, hb * 4:(hb + 1) * 4, :], in0=rbv[hb],
                                    in1=cum[:, hb * 4:(hb + 1) * 4].unsqueeze(2).to_broadcast([128, 4, 128]), op=ALU.subtract),
                                    reads=[("ps", p_rb[hb]), "cum"], writes=["seg"])
                            S.op("dve", lambda e: e.tensor_scalar(out=seg[:], in0=seg[:], scalar1=0.0, scalar2=None, op0=ALU.min),
                                 reads=["seg"], writes=["seg"])
                            S.op("act", lambda e: e.activation(out=Lm[:], in_=seg[:], func=AF.Exp), reads=["seg"], writes=["seg"])
                            p_cb = self.ps_next()
                            S.op("pe", lambda e, p_cb=p_cb, tsl=tsl: e.matmul(self.ps[p_cb][:, 0:128], BT[:, tsl], CT[:, tsl], start=True, stop=True),
                                 reads=["BT", "CT"], writes=[("ps", p_cb)])
                            S.op("dve", lambda e, p_cb=p_cb, MK=MK: e.tensor_tensor(out=cbm[:], in0=self.ps[p_cb][:, 0:128], in1=MK, op=ALU.mult),
                                 reads=[("ps", p_cb), "consts"], writes=["cbm"])
                            S.op("dve", lambda e: e.tensor_tensor(out=MT[:], in0=Lm[:], in1=cbm[:].unsqueeze(1).to_broadcast([128, 8, 128]), op=ALU.mult),
                                 reads=["seg", "cbm"], writes=["MT"])
                        if isx:
                            p_y = self.ps_next()
                            for hh in range(8):
                                S.op("pe", lambda e, p_y=p_y, hh=hh: e.matmul(self.ps[p_y][:, hh * 64:(hh + 1) * 64], MT[:, hh, :], xdt[:, hh, :], start=True, stop=True),
                                     reads=["MT", "xdt"], writes=[("ps", p_y)])
                            S.op("act", lambda e: e.activation(out=ecum[:], in_=cum[:], func=AF.Exp), reads=["cum"], writes=["ecum"])
                            S.op("dve", lambda e, p_ys=p_ys: e.tensor_tensor(
                                out=t1[:].rearrange("p (h q) -> p h q", h=8), in0=self.ps[p_ys][:, :].rearrange("p (h q) -> p h q", h=8),
                                in1=ecum[:].unsqueeze(2).to_broadcast([128, 8, 64]), op=ALU.mult), reads=[("ps", p_ys), "ecum"], writes=["t1"])
                            S.op("dve", lambda e, p_y=p_y: e.tensor_tensor(out=yo[:], in0=self.ps[p_y][:, :], in1=t1[:], op=ALU.add),
                                 reads=[("ps", p_y), "t1"], writes=["yo"])
                            dst = io["s_ys" if d == 0 else "s_ys2"][b, (t - NTC) * 128:(t - NTC + 1) * 128, g * 512:(g + 1) * 512]
                            ykey = ("s_ys", d, t, g)
                            if d == 0:
                                S.op("dve", lambda e, xsv=xsv, g=g: e.tensor_tensor(
                                    out=t1[:].rearrange("p (h q) -> p h q", h=8), in0=xsv,
                                    in1=dsk[:, g * 8:(g + 1) * 8].unsqueeze(2).to_broadcast([128, 8, 64]), op=ALU.mult),
                                    reads=[("xs_tm", t), "dsk"], writes=["t1"])
                                S.op("dve", lambda e: e.tensor_tensor(out=yo[:], in0=yo[:], in1=t1[:], op=ALU.add), reads=["yo", "t1"], writes=["yo"])
                            S.dma(lambda e, dst=dst: e.dma_start(out=dst, in_=yo[:]), reads=["yo"], writes=[ykey])
        S.barrier()

    def rstd_from_ss(self, ss, n, key, width):
        S = self.S
        S.op("dve", lambda e: e.tensor_scalar(out=ss[:, 0:width], in0=ss[:, 0:width], scalar1=1.0 / n, scalar2=EPS, op0=ALU.mult, op1=ALU.add),
             reads=[key], writes=[key])
        S.op("act", lambda e: e.activation(out=ss[:, 0:width], in_=ss[:, 0:width], func=AF.Sqrt), reads=[key], writes=[key])
        S.op("dve", lambda e: e.reciprocal(out=ss[:, 0:width], in_=ss[:, 0:width]), reads=[key], writes=[key])

    def row_bcast(self, dst, dkey, row_ap, rkey, n):
        S = self.S
        for c0 in range(0, n, 512):
            w = min(512, n - c0)
            pb = self.ps_next()
            S.op("pe", lambda e, pb=pb, c0=c0, w=w: e.matmul(self.ps[pb][:, 0:w], self.P["consts"][0:1, 7, :], row_ap[0:1, c0:c0 + w], start=True, stop=True),
                 reads=["consts", rkey], writes=[("ps", pb)])
            S.op("act", lambda e, pb=pb, c0=c0, w=w: e.copy(dst[:, c0:c0 + w], self.ps[pb][:, 0:w]), reads=[("ps", pb)], writes=[dkey])

    def col_to_bcast(self, dst, dkey, which, j, tmp):
        S = self.S
        modT = self.P["modT"]
        S.op("dve", lambda e: e.tensor_tensor(
            out=tmp[:].rearrange("p (k q) -> p k q", k=KC),
            in0=modT[:, which * KC:(which + 1) * KC, j:j + 1].to_broadcast([128, KC, 128]),
            in1=self.cst(0).unsqueeze(1).to_broadcast([128, KC, 128]), op=ALU.mult),
            reads=["modT", "consts"], writes=["c2b_tmp"])
        for c in range(4):
            pb = self.ps_next()
            S.op("pe", lambda e, pb=pb, c=c: e.matmul(self.ps[pb][:, :], self.cst(7), tmp[:, c * 512:(c + 1) * 512], start=True, stop=True),
                 reads=["c2b_tmp", "consts"], writes=[("ps", pb)])
            S.op("act", lambda e, pb=pb, c=c: e.copy(dst[:, c * 512:(c + 1) * 512], self.ps[pb][:, :]), reads=[("ps", pb)], writes=[dkey])

    TB = 2

    def stageD(self, b):
        S, io, P, hT = self.S, self.io, self.P, self.hT
        TB = self.TB
        n = TB * 128
        wv_in = io["w_in"]
        with ExitStack() as es:
            slA = self.sb(es, "slA", [128, KC, 512], BF16)
            slB = self.sb(es, "slB", [128, 32, 512], BF16)
            slG = [self.sb(es, "slG", [128, KC, 512], BF16) for _ in range(1)]
            oT = self.sb(es, "oT", [128, KC, n], BF16)
            yT = self.sb(es, "yT", [128, 32, n], BF16)
            mixT = self.sb(es, "mixT", [128, KC, n], BF16)
            gnwT = self.sb(es, "gnwT", [128, 4])
            snwT = self.sb(es, "snwT", [128, 32])
            bgT = self.sb(es, "bgT", [128, 32])
            g1bc = self.sb(es, "g1bc", [128, D])
            m1b = self.sb(es, "m1b", [128, 4, n])
            rs = self.sb(es, "rs", [128, 512])
            oh = self.sb(es, "oh", [128, 512])
            junk = self.sb(es, "junkd", [128, 512])
            onb = self.sb(es, "onb", [128, 512], BF16)
            ss = self.sb(es, "ssd", [128, 1])
            sa = self.sb(es, "sa", [128, n])
            sb_ = self.sb(es, "sb_", [128, n])
            m1 = self.sb(es, "m1", [128, n])
            m2 = self.sb(es, "m2", [128, n])
            S.dma(lambda e: e.dma_start(out=bgT[:], in_=io["b_gateT"]), writes=["bgT"])
            with ExitStack() as es2:
                tmpbig = self.sb(es2, "tmpbig", [128, D])
                S.dma(lambda e: e.dma_start(out=gnwT[:], in_=io["gnwT"]), writes=["gnwT"])
                S.dma(lambda e: e.dma_start(out=snwT[:], in_=io["snwT"]), writes=["snwT"])
                self.col_to_bcast(g1bc, "g1bc", 2, b, tmpbig)
                S.barrier()
            ptb = self.ps[7][:, :].bitcast(BF16)
            for blk in range(SEQ // n):
                tiles = [NTC + blk * TB + i for i in range(TB)]
                wbufs = [(slA, "slA"), (slG[0], ("slG", 0))]

                def d1_col(part):
                    return (C_R + part * 512) if part < 4 else (C_Z + (part - 4) * 512)

                self.load_w(wbufs[0][0], wbufs[0][1], wv_in, d1_col(0), 512)
                for part in range(12):
                    isg = part < 4
                    cur, curk = wbufs[part % 2]
                    if part + 1 < 12:
                        self.load_w(wbufs[(part + 1) % 2][0], wbufs[(part + 1) % 2][1], wv_in, d1_col(part + 1), 512)
                    for i, t in enumerate(tiles):
                        pb = self.ps_next()
                        for kc in range(KC):
                            S.op("pe", lambda e, pb=pb, kc=kc, t=t, cur=cur: e.matmul(self.ps[pb][:, :], hT[:, kc, t * 128:(t + 1) * 128], cur[:, kc, :],
                                                                                        start=(kc == 0), stop=(kc == KC - 1)),
                                 reads=[curk, ("hT", t)], writes=[("ps", pb)])
                        S.op("act", lambda e, pb=pb: e.activation(out=rs[:], in_=self.ps[pb][:, :], func=AF.Silu), reads=[("ps", pb)], writes=["rs"])
                        tok = slice((t - NTC) * 128, (t - NTC + 1) * 128)
                        if isg:
                            src = io["s_og"][b, tok, part * 512:(part + 1) * 512]
                            src2 = io["s_og2"][b, tok, part * 512:(part + 1) * 512]
                        else:
                            src = io["s_ys"][b, tok, (part - 4) * 512:(part - 3) * 512]
                            src2 = io["s_ys2"][b, tok, (part - 4) * 512:(part - 3) * 512]
                        S.dma(lambda e, src=src: e.dma_start(out=oh[:], in_=src), writes=["oh"])
                        S.dma(lambda e, src2=src2: e.dma_start(out=junk[:], in_=src2), writes=["junkd"])
                        S.op("dve", lambda e: e.tensor_tensor(out=oh[:], in0=oh[:], in1=junk[:], op=ALU.add), reads=["oh", "junkd"], writes=["oh"])
                        if isg:
                            S.op("act", lambda e: e.activation(out=junk[:], in_=oh[:], func=AF.Square, accum_out=ss[:, 0:1]), reads=["oh"], writes=["junkd", "ssd"])
                            self.rstd_from_ss(ss, 512, "ssd", 1)
                            S.op("dve", lambda e: e.scalar_tensor_tensor(out=onb[:], in0=oh[:], scalar=ss[:, 0:1], in1=rs[:], op0=ALU.mult, op1=ALU.mult),
                                 reads=["oh", "ssd", "rs"], writes=["onb"])
                        else:
                            gg = part - 4
                            S.op("dve", lambda e: e.tensor_tensor(out=oh[:], in0=oh[:], in1=rs[:], op=ALU.mult), reads=["oh", "rs"], writes=["oh"])
                            S.op("act", lambda e: e.activation(out=junk[:], in_=oh[:], func=AF.Square, accum_out=ss[:, 0:1]), reads=["oh"], writes=["junkd", "ssd"])
                            self.rstd_from_ss(ss, 512, "ssd", 1)
                            S.op("dve", lambda e: e.tensor_scalar(out=onb[:], in0=oh[:], scalar1=ss[:, 0:1], scalar2=None, op0=ALU.mult),
                                 reads=["oh", "ssd"], writes=["onb"])
                        for q in range(4):
                            S.op("pe", lambda e, q=q: e.transpose(ptb[:, q * 128:(q + 1) * 128], onb[:, q * 128:(q + 1) * 128], self.cstb(0)),
                                 reads=["onb", "cbf"], writes=[("ps", 7)])
                        for q in range(4):
                            if isg:
                                dstT, dk, wcol, wk = oT[:, part * 4 + q, i * 128:(i + 1) * 128], "oT", gnwT[:, q:q + 1], "gnwT"
                            else:
                                kk = (part - 4) * 4 + q
                                dstT, dk, wcol, wk = yT[:, kk, i * 128:(i + 1) * 128], "yT", snwT[:, kk:kk + 1], "snwT"
                            S.op("dve", lambda e, dstT=dstT, wcol=wcol, q=q: e.tensor_scalar(out=dstT, in0=ptb[:, q * 128:(q + 1) * 128], scalar1=wcol, scalar2=None, op0=ALU.mult),
                                 reads=[("ps", 7), wk], writes=[dk])
                hsl = slice(tiles[0] * 128, (tiles[-1] + 1) * 128)
                hk = [("hT", t) for t in tiles]
                self.load_w(slA, "slA", io["w_gla_out"], 0, 512)
                self.load_w(slG[0], ("slG", 0), wv_in, C_GL, 512)
                for s4 in range(4):
                    self.load_w(slB, "slB", io["w_ssd_out"], s4 * 512, 512, nk=32)
                    for q in range(4):
                        cc = s4 * 4 + q
                        cs = slice(q * 128, (q + 1) * 128)
                        p_ya, p_ga = self.ps_next(), self.ps_next()
                        for kc in range(KC):
                            S.op("pe", lambda e, kc=kc, cs=cs, p=p_ya: e.matmul(self.ps[p][:, 0:n], slA[:, kc, cs], oT[:, kc, :], start=(kc == 0), stop=(kc == KC - 1)),
                                 reads=["slA", "oT"], writes=[("ps", p_ya)])
                        for kc in range(KC):
                            S.op("pe", lambda e, kc=kc, cs=cs, p=p_ga, hsl=hsl: e.matmul(self.ps[p][:, 0:n], slG[0][:, kc, cs], hT[:, kc, hsl], start=(kc == 0), stop=(kc == KC - 1)),
                                 reads=[("slG", 0)] + hk, writes=[("ps", p_ga)])
                        S.op("act", lambda e, cc=cc, p=p_ga: e.activation(out=sa[:], in_=self.ps[p][:, 0:n], func=AF.Sigmoid, bias=bgT[:, cc:cc + 1]),
                             reads=[("ps", p_ga), "bgT"], writes=["sa"])
                        S.op("dve", lambda e, p=p_ya, q=q: e.tensor_tensor(out=m1b[:, q, :], in0=self.ps[p][:, 0:n], in1=sa[:], op=ALU.mult),
                             reads=[("ps", p_ya), "sa"], writes=["m1b"])
                    self.load_w(slG[0], ("slG", 0), wv_in, C_GL + D + s4 * 512, 512)
                    if s4 + 1 < 4:
                        self.load_w(slA, "slA", io["w_gla_out"], (s4 + 1) * 512, 512)
                    else:
                        self.load_w(slA, "slA", io["w_o"], 0, 512)
                    for q in range(4):
                        cc = s4 * 4 + q
                        cs = slice(q * 128, (q + 1) * 128)
                        p_yb, p_gb = self.ps_next(), self.ps_next()
                        for kc in range(32):
                            S.op("pe", lambda e, kc=kc, cs=cs, p=p_yb: e.matmul(self.ps[p][:, 0:n], slB[:, kc, cs], yT[:, kc, :], start=(kc == 0), stop=(kc == 31)),
                                 reads=["slB", "yT"], writes=[("ps", p_yb)])
                        for kc in range(KC):
                            S.op("pe", lambda e, kc=kc, cs=cs, p=p_gb, hsl=hsl: e.matmul(self.ps[p][:, 0:n], slG[0][:, kc, cs], hT[:, kc, hsl], start=(kc == 0), stop=(kc == KC - 1)),
                                 reads=[("slG", 0)] + hk, writes=[("ps", p_gb)])
                        S.op("act", lambda e, cc=cc, p=p_gb: e.activation(out=sb_[:], in_=self.ps[p][:, 0:n], func=AF.Sigmoid, bias=bgT[:, KC + cc:KC + cc + 1]),
                             reads=[("ps", p_gb), "bgT"], writes=["sb_"])
                        S.op("dve", lambda e, p=p_yb: e.tensor_tensor(out=m2[:], in0=self.ps[p][:, 0:n], in1=sb_[:], op=ALU.mult), reads=[("ps", p_yb), "sb_"], writes=["m2"])
                        S.op("dve", lambda e, cc=cc, q=q: e.tensor_tensor(out=mixT[:, cc, :], in0=m1b[:, q, :], in1=m2[:], op=ALU.add), reads=["m1b", "m2"], writes=["mixT"])
                    if s4 + 1 < 4:
                        self.load_w(slG[0], ("slG", 0), wv_in, C_GL + (s4 + 1) * 512, 512)
                    else:
                        self.load_w(slG[0], ("slG", 0), io["w_o"], 512, 512)
                for s4 in range(4):
                    cur, curk = wbufs[s4 % 2]
                    for i, t in enumerate(tiles):
                        tok = slice((t - NTC) * 128, (t - NTC + 1) * 128)
                        S.dma(lambda e, tok=tok, s4=s4: e.dma_start(out=oh[:], in_=io["x"][b, tok, s4 * 512:(s4 + 1) * 512]), writes=["oh"])
                        pb = self.ps_next()
                        for kc in range(KC):
                            S.op("pe", lambda e, pb=pb, kc=kc, i=i, cur=cur: e.matmul(self.ps[pb][:, :], mixT[:, kc, i * 128:(i + 1) * 128], cur[:, kc, :],
                                                                                        start=(kc == 0), stop=(kc == KC - 1)),
                                 reads=[curk, "mixT"], writes=[("ps", pb)])
                        S.op("dve", lambda e, pb=pb, s4=s4: e.tensor_tensor(out=rs[:], in0=self.ps[pb][:, :], in1=g1bc[:, s4 * 512:(s4 + 1) * 512], op=ALU.mult),
                             reads=[("ps", pb), "g1bc"], writes=["rs"])
                        S.op("dve", lambda e: e.tensor_tensor(out=junk[:], in0=oh[:], in1=rs[:], op=ALU.add), reads=["oh", "rs"], writes=["junkd"])
                        S.dma(lambda e, tok=tok, s4=s4: e.dma_start(out=io["s_x1"][b, tok, s4 * 512:(s4 + 1) * 512], in_=junk[:]),
                              reads=["junkd"], writes=[("s_x1", b, t, s4)])
                    if s4 + 2 < 4:
                        self.load_w(cur, curk, io["w_o"], (s4 + 2) * 512, 512)
        S.barrier()

    def stageE(self, b):
        S, io, P = self.S, self.io, self.P
        TB = self.TB
        n = TB * 128
        NEG = -1.0e30
        EW = 512
        NEB = 16384 // EW
        with ExitStack() as es:
            fnw_bc = self.sb(es, "fnw_bc", [128, D])
            g2bc = self.sb(es, "g2bc", [128, D])
            with ExitStack() as es2:
                fnw_r = self.sb(es2, "fnw_r", [1, D])
                tmpbig = self.sb(es2, "tmpbigE", [128, D])
                S.dma(lambda e: e.dma_start(out=fnw_r[:], in_=io["fnw"]), writes=["fnw_r"])
                self.row_bcast(fnw_bc, "fnw_bc", fnw_r, "fnw_r", D)
                self.col_to_bcast(g2bc, "g2bc", 5, b, tmpbig)
                S.barrier()
            slU = [self.sb(es, "slU", [128, KC, EW], BF16) for _ in range(2)]
            slV = [self.sb(es, "slV", [128, 4, D], BF16) for _ in range(2)]
            keysT = self.sb(es, "keysT", [128, 16, 128])
            x1b = self.sb(es, "x1e", [128, D])
            x1 = [x1b for _ in range(TB)]
            yacc = [self.sb(es, "yacc", [128, D]) for _ in range(TB)]
            hx2T = self.sb(es, "hx2T", [128, KC, n], BF16)
            qTp = self.sb(es, "qTp", [128, 16, n])
            sc = [self.sb(es, "sc", [128, 16, 128]) for _ in range(TB)]
            thr = [self.sb(es, "thr", [128, 8]) for _ in range(TB)]
            c0t = [self.sb(es, "c0t", [128, 8]) for _ in range(TB)]
            sv = self.sb(es, "sv", [128, 16, 16])
            tmp128 = self.sb(es, "tmp128", [128, 128])
            cand = self.sb(es, "cand", [128, 256])
            cand2 = self.sb(es, "cand2", [128, 256])
            cv = self.sb(es, "cv", [128, 16])
            junk16 = self.sb(es, "junk16", [128, 16])
            negm = self.sb(es, "negm", [128, 8])
            Zs = self.sb(es, "Zs", [128, 8])
            gA = [self.sb(es, "gA", [128, EW], BF16) for _ in range(2)]
            candb = [self.sb(es, "candb", [128, EW]) for _ in range(4)]
            EE = [self.sb(es, "EE", [128, EW], BF16) for _ in range(4)]
            Ghs = [[self.sb(es, "Ghs", [128, EW], BF16) for _ in range(8)] for _ in range(2)]
            WT = [self.sb(es, "WT", [128, 4, 128], BF16) for _ in range(2)]
            ntmp = {"xs": self.sb(es, "xsE", [128, D]), "junk": self.sb(es, "junkE", [128, D], BF16), "ss": self.sb(es, "ssE", [128, 4])}
            ssf = self.sb(es, "ssf", [128, 1])
            S.dma(lambda e: e.dma_start(out=keysT[:], in_=io["keysT"]), writes=["keysT"])
            uT_bf, ukeys = self.wsrc(io["uT"])
            pv_bf, vkeys = self.wsrc(io["pv"])
            uview = uT_bf.rearrange("(kc p) c -> p kc c", p=128)

            def load_U(eb):
                sl = eb % 2
                S.dma(lambda e, eb=eb, sl=sl: e.dma_start(out=slU[sl][:], in_=uview[:, :, eb * EW:(eb + 1) * EW]),
                      reads=ukeys, writes=[("slU", sl)], q="sp")

            def load_V(eb):
                sl = eb % 2
                S.dma(lambda e, eb=eb, sl=sl: e.dma_start(out=slV[sl][:], in_=pv_bf[eb * EW:(eb + 1) * EW, :].rearrange("(ec p) c -> p ec c", p=128)),
                      reads=vkeys, writes=[("slV", sl)], q="sp")

            def load_slabs(eb):
                load_U(eb)
                load_V(eb)

            for blk in range(SEQ // n):
                toks = [blk * TB + i for i in range(TB)]
                self.ps_rot = [0, 1, 2, 3]
                for i, t in enumerate(toks):
                    S.dma(lambda e, i=i, t=t: e.dma_start(out=x1[i][:], in_=io["s_x1"][b, t * 128:(t + 1) * 128, :]), writes=["x1e"])
                    self.norm_to_T(es, "x1e", x1[i][:], P["A2"], 3, b,
                                   lambda kc, i=i: hx2T[:, kc, i * 128:(i + 1) * 128], ["hx2T"], ntmp)
                for s4 in range(4):
                    sl = s4 % 2
                    self.load_w(slU[sl], ("slU", sl), io["wq"], s4 * 512, 512)
                    for q in range(4):
                        j = s4 * 4 + q
                        pb = self.ps_next()
                        for kc in range(KC):
                            S.op("pe", lambda e, pb=pb, kc=kc, q=q, sl=sl: e.matmul(self.ps[pb][:, 0:n], slU[sl][:, kc, q * 128:(q + 1) * 128], hx2T[:, kc, :],
                                                                                     start=(kc == 0), stop=(kc == KC - 1)), reads=[("slU", sl), "hx2T"], writes=[("ps", pb)])
                        S.op("act", lambda e, pb=pb, j=j: e.copy(qTp[:, j, :], self.ps[pb][:, 0:n]), reads=[("ps", pb)], writes=["qTp"])
                load_slabs(0)
                for i in range(TB):
                    for j4 in range(4):
                        pb = self.ps_next()
                        for q in range(4):
                            j = j4 * 4 + q
                            S.op("pe", lambda e, pb=pb, q=q, j=j, i=i: e.matmul(self.ps[pb][:, q * 128:(q + 1) * 128], qTp[:, j, i * 128:(i + 1) * 128], keysT[:, j, :],
                                                                                 start=True, stop=True), reads=["qTp", "keysT"], writes=[("ps", pb)])
                        S.op("act", lambda e, pb=pb, j4=j4, i=i: e.copy(sc[i][:, j4 * 4:(j4 + 1) * 4, :], self.ps[pb][:, :].rearrange("p (q k) -> p q k", q=4)),
                             reads=[("ps", pb)], writes=[("sc", i)])
                    for j in range(16):
                        S.op("dve", lambda e, j=j, i=i: e.max(out=sv[:, j, 0:8], in_=sc[i][:, j, :]), reads=[("sc", i)], writes=["sv"])
                        S.op("dve", lambda e, j=j, i=i: e.match_replace(out=tmp128[:], in_to_replace=sv[:, j, 0:8], in_values=sc[i][:, j, :], imm_value=NEG),
                             reads=[("sc", i), "sv"], writes=["tmp128"])
                        S.op("dve", lambda e, j=j: e.max(out=sv[:, j, 8:16], in_=tmp128[:]), reads=["tmp128"], writes=["sv"])
                    for h in range(8):
                        S.op("pool", lambda e, h=h: e.tensor_tensor(
                            out=cand[:].rearrange("p (a c) -> p a c", a=16),
                            in0=sv[:, 2 * h, :].unsqueeze(2).to_broadcast([128, 16, 16]),
                            in1=sv[:, 2 * h + 1, :].unsqueeze(1).to_broadcast([128, 16, 16]), op=ALU.add), reads=["sv"], writes=["cand"])
                        S.op("dve", lambda e: e.max(out=cv[:, 0:8], in_=cand[:]), reads=["cand"], writes=["cv"])
                        S.op("dve", lambda e: e.match_replace(out=cand2[:], in_to_replace=cv[:, 0:8], in_values=cand[:], imm_value=NEG),
                             reads=["cand", "cv"], writes=["cand2"])
                        S.op("dve", lambda e: e.max(out=cv[:, 8:16], in_=cand2[:]), reads=["cand2"], writes=["cv"])
                        S.op("dve", lambda e, h=h, i=i: e.tensor_copy(thr[i][:, h:h + 1], cv[:, 15:16]), reads=["cv"], writes=[("thr", i)])
                        S.op("dve", lambda e, h=h: e.tensor_scalar(out=negm[:, h:h + 1], in0=cv[:, 0:1], scalar1=-1.0, scalar2=None, op0=ALU.mult),
                             reads=["cv"], writes=["negm"])
                        S.op("act", lambda e, h=h: e.activation(out=junk16[:], in_=cv[:], func=AF.Exp, bias=negm[:, h:h + 1], accum_out=Zs[:, h:h + 1]),
                             reads=["cv", "negm"], writes=["junk16", "Zs"])
                    S.op("act", lambda e: e.activation(out=Zs[:], in_=Zs[:], func=AF.Ln), reads=["Zs"], writes=["Zs"])
                    S.op("dve", lambda e, i=i: e.tensor_tensor(out=c0t[i][:], in0=negm[:], in1=Zs[:], op=ALU.subtract), reads=["negm", "Zs"], writes=[("c0t", i)])
                pairs = [(eb, i) for eb in range(NEB) for i in range(TB)]
                ptb = [self.ps[2][:, :].bitcast(BF16), self.ps[3][:, :].bitcast(BF16)]

                def emit_A(pi):
                    eb, i = pairs[pi]
                    pa = pi % 2
                    sl = eb % 2
                    for ec in range(4):
                        for kc in range(KC):
                            S.op("pe", lambda e, pa=pa, kc=kc, i=i, sl=sl, ec=ec: e.matmul(
                                self.ps[pa][:, ec * 128:(ec + 1) * 128], slU[sl][:, kc, ec * 128:(ec + 1) * 128], hx2T[:, kc, i * 128:(i + 1) * 128],
                                start=(kc == 0), stop=(kc == KC - 1)), reads=["hx2T", ("slU", sl)], writes=[("ps", pa)])

                def emit_gelu(pi):
                    pa = pi % 2
                    S.op("act", lambda e, pa=pa: e.activation(out=gA[pa][:], in_=self.ps[pa][:, :], func=AF.Gelu), reads=[("ps", pa)], writes=[("gA", pa)])

                def tail_T(pj):
                    pa = pj % 2
                    for ec in range(4):
                        for h in range(8):
                            S.op("pe", lambda e, ec=ec, pa=pa, h=h: e.matmul(
                                self.ps[2 + pa][:, ec * 128:(ec + 1) * 128], Ghs[pa][h][:, ec * 128:(ec + 1) * 128], self.cstb(0),
                                start=(h == 0), stop=(h == 7)), reads=[("Ghs", pa, h), "cbf"], writes=[("ps", 2 + pa)])
                    S.op("dve", lambda e, pa=pa: e.tensor_tensor(out=WT[pa][:].rearrange("p c t -> p (c t)"), in0=self.ps[2 + pa][:, :], in1=gA[pa][:], op=ALU.mult),
                         reads=[("ps", 2 + pa), ("gA", pa)], writes=[("WT", pa)])

                def tail_y(pj):
                    eb, i = pairs[pj]
                    pa = pj % 2
                    sl = eb % 2
                    for cb4 in range(4):
                        for ec in range(4):
                            S.op("pe", lambda e, cb4=cb4, ec=ec, pa=pa, sl=sl: e.matmul(self.ps[4 + cb4][:, :], WT[pa][:, ec, :], slV[sl][:, ec, cb4 * 512:(cb4 + 1) * 512],
                                                                                       start=(ec == 0), stop=(ec == 3)), reads=[("WT", pa), ("slV", sl)], writes=[("ps", 4 + cb4)])

                def tail_acc(pj):
                    eb, i = pairs[pj]
                    for cb4 in range(4):
                        if eb == 0:
                            S.op("act", lambda e, cb4=cb4, i=i: e.copy(yacc[i][:, cb4 * 512:(cb4 + 1) * 512], self.ps[4 + cb4][:, :]),
                                 reads=[("ps", 4 + cb4)], writes=[("yacc", i, cb4)])
                        else:
                            S.op("dve", lambda e, cb4=cb4, i=i: e.tensor_tensor(out=yacc[i][:, cb4 * 512:(cb4 + 1) * 512], in0=yacc[i][:, cb4 * 512:(cb4 + 1) * 512],
                                                                                   in1=self.ps[4 + cb4][:, :], op=ALU.add), reads=[("ps", 4 + cb4), ("yacc", i, cb4)], writes=[("yacc", i, cb4)])

                emit_A(0)
                emit_gelu(0)
                for pi, (eb, i) in enumerate(pairs):
                    pa = pi % 2
                    sl = eb % 2
                    if i == 0 and eb + 1 < NEB:
                        load_U(eb + 1)
                    for h in range(8):
                        hb = h % 4
                        ceng = "pool"
                        S.op(ceng, lambda e, h=h, i=i, eb=eb, hb=hb: e.tensor_tensor(
                            out=candb[hb][:].rearrange("p (a k) -> p a k", a=4),
                            in0=sc[i][:, 2 * h, eb * 4:(eb + 1) * 4].unsqueeze(2).to_broadcast([128, 4, 128]),
                            in1=sc[i][:, 2 * h + 1, :].unsqueeze(1).to_broadcast([128, 4, 128]), op=ALU.add), reads=[("sc", i)], writes=[("candb", hb)])
                        S.op("act", lambda e, h=h, i=i, hb=hb: e.activation(out=EE[hb][:], in_=candb[hb][:], func=AF.Exp, bias=c0t[i][:, h:h + 1]),
                             reads=[("candb", hb), ("c0t", i)], writes=[("EE", hb)])
                        S.op("dve", lambda e, h=h, i=i, hb=hb, pa=pa: e.scalar_tensor_tensor(out=Ghs[pa][h][:], in0=candb[hb][:], scalar=thr[i][:, h:h + 1], in1=EE[hb][:],
                                                                                             op0=ALU.is_ge, op1=ALU.mult), reads=[("candb", hb), ("EE", hb), ("thr", i)], writes=[("Ghs", pa, h)])
                        if h == 1:
                            if pi > 0:
                                tail_T(pi - 1)
                            if pi + 1 < len(pairs):
                                emit_A(pi + 1)
                        if h == 3:
                            if pi > 0:
                                tail_y(pi - 1)
                            if i == 0 and eb + 1 < NEB:
                                load_V(eb + 1)
                        if pi > 0 and h == 6:
                            tail_acc(pi - 1)
                    if pi + 1 < len(pairs):
                        emit_gelu(pi + 1)
                last = len(pairs) - 1
                tail_T(last)
                tail_y(last)
                tail_acc(last)
                self.ps_rot = list(range(8))
                for i, t in enumerate(toks):
                    S.dma(lambda e, i=i, t=t: e.dma_start(out=x1[i][:], in_=io["s_x1"][b, t * 128:(t + 1) * 128, :]), writes=["x1e"])
                    S.op("dve", lambda e, i=i: e.tensor_tensor(out=yacc[i][:], in0=yacc[i][:], in1=g2bc[:], op=ALU.mult), reads=[("yacc", i, 0), ("yacc", i, 1), ("yacc", i, 2), ("yacc", i, 3), "g2bc"], writes=[("yacc", i, 0), ("yacc", i, 1), ("yacc", i, 2), ("yacc", i, 3)])
                    S.op("dve", lambda e, i=i: e.tensor_tensor(out=x1[i][:], in0=x1[i][:], in1=yacc[i][:], op=ALU.add), reads=[("yacc", i, 0), ("yacc", i, 1), ("yacc", i, 2), ("yacc", i, 3), "x1e"], writes=["x1e"])
                    S.op("act", lambda e, i=i: e.activation(out=yacc[i][:], in_=x1[i][:], func=AF.Square, accum_out=ssf[:, 0:1]), reads=["x1e"], writes=[("yacc", i, 0), ("yacc", i, 1), ("yacc", i, 2), ("yacc", i, 3), "ssf"])
                    self.rstd_from_ss(ssf, D, "ssf", 1)
                    S.op("dve", lambda e, i=i: e.scalar_tensor_tensor(out=yacc[i][:], in0=x1[i][:], scalar=ssf[:, 0:1], in1=fnw_bc[:], op0=ALU.mult, op1=ALU.mult),
                         reads=["x1e", "ssf", "fnw_bc"], writes=[("yacc", i, 0), ("yacc", i, 1), ("yacc", i, 2), ("yacc", i, 3)])
                    S.dma(lambda e, i=i, t=t: e.dma_start(out=io["out"][b, t * 128:(t + 1) * 128, :], in_=yacc[i][:]), reads=[("yacc", i, 0), ("yacc", i, 1), ("yacc", i, 2), ("yacc", i, 3)], writes=[("out", b, t)])
        S.barrier()


def _consts():
    t = np.arange(128)[:, None]
    i = np.arange(128)[None, :]
    c = np.zeros((128, 8, 128), np.float32)
    c[:, 0] = (t == i)
    c[:, 1] = (t <= i)
    c[:, 2] = (t >= i)
    c[:, 3] = (t > i)
    c[:, 4] = (t < i)
    c[:, 5] = (t <= i)
    c[:, 6] = (t >= i)
    c[:, 7] = 1.0
    return c


def _col(v, n):
    return np.ascontiguousarray(np.asarray(v, np.float32).reshape(n, 128).T)


def make_in_maps(inputs, n_cores=8, nb=2):
    g = {k: np.asarray(v) for k, v in inputs.items()}
    shared = {
        "w_ada": np.ascontiguousarray(g["w_ada"][0]),
        "b_adaT": _col(g["b_ada"][0], 96),
        "n1T": _col(g["norm1_w"][0], KC),
        "n2T": _col(g["norm2_w"][0], KC),
        "w_in": np.ascontiguousarray(g["w_in"][0]),
        "w2f": np.ascontiguousarray(np.concatenate([g["w_lr2_f"][0], g["b_lr_f"][0][None]], 0)),
        "w2b": np.ascontiguousarray(np.concatenate([g["w_lr2_b"][0], g["b_lr_b"][0][None]], 0)),
        "gnwT": _col(g["gla_norm_w"][0], 4),
        "snwT": _col(g["ssd_norm_w"][0], 32),
        "fnw": np.ascontiguousarray(g["final_norm_w"][None]),
        "conv_wT": np.ascontiguousarray(g["conv_w"][0].reshape(9, 48, 128).transpose(2, 1, 0)),
        "conv_bT": _col(g["conv_b"][0], 48),
        "ssd_rows": np.ascontiguousarray(np.concatenate(
            [g["a_log_f"][0], g["a_log_b"][0], g["d_skip"][0], g["dt_bias_f"][0], g["dt_bias_b"][0]])[None]),
        "b_gateT": _col(g["b_gate"][0], 32),
        "w_gla_out": np.ascontiguousarray(g["w_gla_out"][0]),
        "w_ssd_out": np.ascontiguousarray(g["w_ssd_out"][0]),
        "w_o": np.ascontiguousarray(g["w_o"][0]),
        "wq": np.ascontiguousarray(g["peer_wq"][0]),
        "keysT": np.ascontiguousarray(g["peer_keys"][0].reshape(16, 128, 128).transpose(2, 0, 1)),
        "uT": np.ascontiguousarray(g["peer_u"][0].T),
        "pv": np.ascontiguousarray(g["peer_v"][0]),
        "consts": _consts(),
    }
    maps = []
    for i in range(n_cores):
        bs = slice(i * nb, (i + 1) * nb)
        cvecs = np.stack([g["c"][i * nb + k] for k in range(nb)] + [g["c_ctx"]] * (3 - nb), -1)
        m = dict(shared)
        m["x"] = np.ascontiguousarray(g["x"][bs])
        m["ctx"] = np.ascontiguousarray(g["ctx"][bs])
        m["cT"] = np.ascontiguousarray(cvecs.reshape(KC, 128, 3).transpose(1, 0, 2).astype(np.float32))
        maps.append(m)
    return maps


def kernel(**inputs):
    n = 8
    nc = K(nb=2).build()
    maps = make_in_maps(inputs, n, 2)
    res = run_bass_kernel_spmd(nc, maps, core_ids=list(range(n)))
    return np.concatenate([r["out"] for r in res.results], axis=0).astype(np.float32)
```
